# Optimizing a Trainium2 kernel written in Bass

```python
import jax, jax.numpy as jnp
from jax import lax
import numpy as np

D_MODEL = 2048
BATCH = 4
SEQ = 4096
DEPTH = 1

CONV_WIDTH = 1024
CONV_KERNEL = 31
N_HEADS = 8
N_KV_HEADS = 2
HEAD_DIM = 128
ATTN_WIDTH = N_HEADS * HEAD_DIM
KV_WIDTH = N_KV_HEADS * HEAD_DIM
IDX_HEADS = 16
IDX_DIM = 64
TOPK_MAX = 256
ROPE_THETA = 500000.0
ROPE_FRACTION_DIV = 4
D_FF = 5632
Q_BLOCK = 128
NORM_EPS = 1e-6
N_BRANCHES = 2
IN_SPLITS = (2 * CONV_WIDTH, ATTN_WIDTH, KV_WIDTH, KV_WIDTH, IDX_HEADS * IDX_DIM, IDX_DIM, IDX_HEADS, N_BRANCHES * D_MODEL)
IN_WIDTH = 2 * CONV_WIDTH + ATTN_WIDTH + 2 * KV_WIDTH + IDX_HEADS * IDX_DIM + IDX_DIM + IDX_HEADS + N_BRANCHES * D_MODEL

kernel_name = "hybrid_gated_conv_dsa_macaron"


def rms_norm(x, g):
    xf = x.astype(jnp.float32)
    y = xf * lax.rsqrt(jnp.mean(xf * xf, axis=-1, keepdims=True) + NORM_EPS)
    return (y * g.astype(jnp.float32)).astype(x.dtype)


def layer_norm(x, g, b):
    xf = x.astype(jnp.float32)
    mu = jnp.mean(xf, axis=-1, keepdims=True)
    xc = xf - mu
    y = xc * lax.rsqrt(jnp.mean(xc * xc, axis=-1, keepdims=True) + NORM_EPS)
    return (y * g.astype(jnp.float32) + b.astype(jnp.float32)).astype(x.dtype)


def swiglu_ffn(x, w1, w2):
    gate, up = jnp.split(x @ w1, 2, axis=-1)
    return (jax.nn.silu(gate) * up) @ w2


def partial_rope(x, positions):
    dh = x.shape[-1]
    r = dh // ROPE_FRACTION_DIV
    half = r // 2
    inv_freq = jnp.power(jnp.float32(ROPE_THETA), -jnp.arange(half, dtype=jnp.float32) * (2.0 / r))
    ang = positions.astype(jnp.float32)[..., None] * inv_freq
    cos = jnp.cos(ang)[:, :, None, :].astype(x.dtype)
    sin = jnp.sin(ang)[:, :, None, :].astype(x.dtype)
    x1 = x[..., :half]
    x2 = x[..., half:r]
    return jnp.concatenate([x1 * cos - x2 * sin, x2 * cos + x1 * sin, x[..., r:]], axis=-1)


def split_columns(z):
    bounds = np.cumsum(np.array(IN_SPLITS))[:-1].tolist()
    return jnp.split(z, bounds, axis=-1)


def conformer_conv(z_glu, dw, dw_b, ln_g, ln_b, w_pw):
    a, b = jnp.split(z_glu, 2, axis=-1)
    u = a * jax.nn.sigmoid(b)
    u = lax.conv_general_dilated(
        u, dw[:, None, :].astype(u.dtype), window_strides=(1,), padding=[(CONV_KERNEL - 1, 0)],
        dimension_numbers=("NWC", "WIO", "NWC"), feature_group_count=CONV_WIDTH) + dw_b
    u = jax.nn.silu(layer_norm(u, ln_g, ln_b))
    return u @ w_pw


def dsa_attention(q, k, v, qi, ki, wi):
    B, T = q.shape[0], q.shape[1]
    topk = min(TOPK_MAX, T // 4)
    n_blocks = T // Q_BLOCK
    key_pos = jnp.arange(T, dtype=jnp.int32)
    qg = q.reshape(B, T, N_KV_HEADS, N_HEADS // N_KV_HEADS, HEAD_DIM)
    gather_rows = jax.vmap(lambda table, idx: table[idx])
    idx_scale = IDX_DIM ** -0.5
    attn_scale = HEAD_DIM ** -0.5

    def block(i):
        start = i * Q_BLOCK
        q_pos = start + jnp.arange(Q_BLOCK, dtype=jnp.int32)
        qb = lax.dynamic_slice_in_dim(qg, start, Q_BLOCK, axis=1)
        qib = lax.dynamic_slice_in_dim(qi, start, Q_BLOCK, axis=1)
        wib = lax.dynamic_slice_in_dim(wi, start, Q_BLOCK, axis=1)
        dots = jnp.einsum("bqhd,bsd->bqhs", qib, ki).astype(jnp.float32) * idx_scale
        scores = jnp.einsum("bqh,bqhs->bqs", wib.astype(jnp.float32), jax.nn.relu(dots))
        causal = key_pos[None, :] <= q_pos[:, None]
        scores = jnp.where(causal[None], scores, -jnp.inf)
        _, sel = lax.top_k(scores, topk)
        ks = gather_rows(k, sel)
        vs = gather_rows(v, sel)
        logits = jnp.einsum("bqgrd,bqkgd->bqgrk", qb, ks).astype(jnp.float32) * attn_scale
        valid = sel <= q_pos[None, :, None]
        logits = jnp.where(valid[:, :, None, None, :], logits, -jnp.inf)
        p = jax.nn.softmax(logits, axis=-1).astype(vs.dtype)
        o = jnp.einsum("bqgrk,bqkgd->bqgrd", p, vs)
        return o.reshape(B, Q_BLOCK, ATTN_WIDTH)

    out = lax.map(block, jnp.arange(n_blocks, dtype=jnp.int32))
    return out.transpose(1, 0, 2, 3).reshape(B, T, ATTN_WIDTH)


def setup_inputs(seed: int = 0) -> dict:
    key = jax.random.key(seed)
    ks = jax.random.split(key, 24)
    L = DEPTH

    def w(k, shape, fan_in):
        return jax.random.normal(k, shape, jnp.float32) * (fan_in ** -0.5)

    def gain(k, n):
        return 1.0 + 0.02 * jax.random.normal(k, (L, n), jnp.float32)

    def bias(k, n):
        return 0.02 * jax.random.normal(k, (L, n), jnp.float32)

    x = jax.random.normal(ks[0], (BATCH, SEQ, D_MODEL), jnp.float32)
    offs = jax.random.randint(ks[1], (BATCH, 1), 0, 1024, dtype=jnp.int32)
    positions = (jnp.arange(SEQ, dtype=jnp.int32)[None, :] + offs).astype(jnp.int32)
    return {
        "x": x,
        "positions": positions,
        "ffn1_norm_pre": gain(ks[2], D_MODEL),
        "ffn1_w1": w(ks[3], (L, D_MODEL, 2 * D_FF), D_MODEL),
        "ffn1_w2": w(ks[4], (L, D_FF, D_MODEL), D_FF),
        "ffn1_norm_post": gain(ks[5], D_MODEL),
        "mix_norm_pre": gain(ks[6], D_MODEL),
        "w_in": w(ks[7], (L, D_MODEL, IN_WIDTH), D_MODEL),
        "conv_dw": w(ks[8], (L, CONV_KERNEL, CONV_WIDTH), CONV_KERNEL),
        "conv_dw_b": bias(ks[9], CONV_WIDTH),
        "conv_ln_g": gain(ks[10], CONV_WIDTH),
        "conv_ln_b": bias(ks[11], CONV_WIDTH),
        "conv_w_pw": w(ks[12], (L, CONV_WIDTH, D_MODEL), CONV_WIDTH),
        "attn_w_o": w(ks[13], (L, ATTN_WIDTH, D_MODEL), ATTN_WIDTH),
        "w_out": w(ks[14], (L, D_MODEL, D_MODEL), D_MODEL),
        "mix_norm_post": gain(ks[15], D_MODEL),
        "ffn2_norm_pre": gain(ks[16], D_MODEL),
        "ffn2_w1": w(ks[17], (L, D_MODEL, 2 * D_FF), D_MODEL),
        "ffn2_w2": w(ks[18], (L, D_FF, D_MODEL), D_FF),
        "ffn2_norm_post": gain(ks[19], D_MODEL),
    }


def reference(x, positions, ffn1_norm_pre, ffn1_w1, ffn1_w2, ffn1_norm_post, mix_norm_pre, w_in,
              conv_dw, conv_dw_b, conv_ln_g, conv_ln_b, conv_w_pw, attn_w_o, w_out, mix_norm_post,
              ffn2_norm_pre, ffn2_w1, ffn2_w2, ffn2_norm_post):
    B, T, _ = x.shape
    for l in range(DEPTH):
        x = x + 0.5 * rms_norm(swiglu_ffn(rms_norm(x, ffn1_norm_pre[l]), ffn1_w1[l], ffn1_w2[l]), ffn1_norm_post[l])

        h = rms_norm(x, mix_norm_pre[l])
        z = h @ w_in[l]
        z_conv, z_q, z_k, z_v, z_qi, z_ki, z_wi, z_gate = split_columns(z)

        y_conv = conformer_conv(z_conv, conv_dw[l], conv_dw_b[l], conv_ln_g[l], conv_ln_b[l], conv_w_pw[l])

        q = partial_rope(z_q.reshape(B, T, N_HEADS, HEAD_DIM), positions)
        k = partial_rope(z_k.reshape(B, T, N_KV_HEADS, HEAD_DIM), positions)
        v = z_v.reshape(B, T, N_KV_HEADS, HEAD_DIM)
        qi = partial_rope(z_qi.reshape(B, T, IDX_HEADS, IDX_DIM), positions)
        ki = partial_rope(z_ki.reshape(B, T, 1, IDX_DIM), positions)[:, :, 0, :]
        wi = z_wi * (IDX_HEADS ** -0.5)
        y_attn = dsa_attention(q, k, v, qi, ki, wi) @ attn_w_o[l]

        gates = jax.nn.sigmoid(z_gate.reshape(B, T, N_BRANCHES, D_MODEL))
        merged = gates[:, :, 0, :] * y_conv + gates[:, :, 1, :] * y_attn
        x = x + rms_norm(merged @ w_out[l], mix_norm_post[l])

        x = x + 0.5 * rms_norm(swiglu_ffn(rms_norm(x, ffn2_norm_pre[l]), ffn2_w1[l], ffn2_w2[l]), ffn2_norm_post[l])
    return x
```

```python
import numpy as np
from contextlib import ExitStack
import concourse.bass as bass
import concourse.mybir as mybir
from concourse.bass_utils import run_bass_kernel_spmd
from concourse.alu_op_type import AluOpType as ALU

F32 = mybir.dt.float32
BF16 = mybir.dt.bfloat16
I32 = mybir.dt.int32
AF = mybir.ActivationFunctionType
AX = mybir.AxisListType

D = 2048
DFF = 5632
NFF = DFF // 128
C = 512
NPRE = 4
NOWN = 4
INW = 8784
EPS = 1e-6
NIT = 16
NR = 3
MAGIC = 12582912.0
ATT_SCALE = 128 ** -0.5
TWO_PI = 6.283185307179586

G_F1PRE, G_F1POST, G_MIXPRE, G_MIXPOST, G_F2PRE, G_F2POST = 0, 16, 32, 48, 64, 80
C_DW = 96
C_DWB = C_DW + 248
C_LNG = C_DWB + 8
C_LNB = C_LNG + 8
C_INVF = C_LNB + 8
C_VFLAG = C_INVF + 2
C_NEGBIG = C_VFLAG + 1
C_BIS = C_NEGBIG + 1
NCST = C_BIS + NIT + 1


class Tr:
    __slots__ = ("w", "r")

    def __init__(self):
        self.w = None
        self.r = {}


class Prog:
    ENG = ("pe", "act", "dve", "pool", "sp")

    def __init__(self):
        self.ops = {e: [] for e in self.ENG}
        self.cnt = {e: 0 for e in self.ENG}
        self.seen = {e: {} for e in self.ENG}
        self.dtot = {}

    def _need(self, eng, reads, writes):
        need = {}

        def add(s, v):
            if need.get(s, 0) < v:
                need[s] = v

        own = 0
        for t in reads:
            if t.w is not None:
                add(*t.w)
                if t.w[0] == eng:
                    own = max(own, t.w[1])
        for t in writes:
            if t.w is not None:
                add(*t.w)
            for s, v in t.r.items():
                add(s, v)
        out = []
        seen = self.seen[eng]
        for s, v in need.items():
            if s == eng:
                continue
            if seen.get(s, 0) < v:
                out.append((s, v))
                seen[s] = v
        if eng != "pe" and own > seen.get(eng, 0):
            out.append((eng, own))
            seen[eng] = own
        return out

    def op(self, eng, fn, reads=(), writes=()):
        waits = self._need(eng, reads, writes)
        self.cnt[eng] += 1
        v = self.cnt[eng]
        for t in reads:
            if t.r.get(eng, 0) < v:
                t.r[eng] = v
        for t in writes:
            t.w = (eng, v)
            t.r = {}
        self.ops[eng].append((waits, fn, eng, 1))

    def dma(self, eng, dsem, fn, reads=(), writes=(), n=1):
        waits = self._need(eng, reads, writes)
        self.dtot[dsem] = self.dtot.get(dsem, 0) + 16 * n
        v = self.dtot[dsem]
        for t in reads:
            if t.r.get(dsem, 0) < v:
                t.r[dsem] = v
        for t in writes:
            t.w = (dsem, v)
            t.r = {}
        self.ops[eng].append((waits, fn, dsem, 16))


def build_nc():
    nc = bass.Bass("TRN2", target_bir_lowering=False)
    P = Prog()

    def din(name, shape, dt=F32):
        return nc.dram_tensor(name, list(shape), dt, kind="ExternalInput").ap()

    xin = din("xin", [(NPRE + NOWN) * C, D])
    posb = din("posb", [128, (NPRE + NOWN) * C], I32)
    cstd = din("cst", [128, NCST])
    rmatd = din("rmat", [128, 256])
    W1a = din("ffn1_w1", [D, 2 * DFF])
    W2a = din("ffn1_w2", [DFF, D])
    Win = din("w_in", [D, INW])
    Wpw = din("conv_w_pw", [1024, D])
    Wo = din("attn_w_o", [1024, D])
    Wout = din("w_out", [D, D])
    W1b = din("ffn2_w1", [D, 2 * DFF])
    W2b = din("ffn2_w2", [DFF, D])
    outd = nc.dram_tensor("out", [NOWN * C, D], F32, kind="ExternalOutput").ap()

    es = ExitStack()

    def sb(name, shape, dt):
        return es.enter_context(nc.sbuf_tensor(name, list(shape), dt))

    ringall = sb("ringall", [128, NR, 4096], BF16)
    ring = [ringall[:, i, :] for i in range(NR)]
    ring_tr = [Tr() for _ in range(NR)]
    xres = sb("xres", [128, 16, C], F32)
    xres_tr = [Tr() for _ in range(16)]
    hT = sb("hT", [128, 16, C], BF16)
    hT_tr = [Tr() for _ in range(16)]
    aT = sb("aT", [128, NFF, C], BF16)
    aT_tr = [Tr() for _ in range(NFF)]
    mixs = sb("mixs", [128, 12, C], F32)
    mix_tr = [Tr() for _ in range(12)]
    Kc = sb("Kc", [128, 2, 4096], BF16)
    Kc_tr = Tr()
    Vc = sb("Vc", [128, 32, 256], BF16)
    Vc_tr = Tr()
    Kic = sb("Kic", [128, 4096], BF16)
    Kic_tr = Tr()
    ubuf = sb("ubuf", [128, 8, 32 + C], BF16)
    ub_tr = [Tr() for _ in range(8)]
    cst = sb("cst_sb", [128, NCST], F32)
    cst_tr = Tr()
    gh = sb("gh", [128, 32], F32)
    gh_tr = Tr()
    rmat = sb("rmat_sb", [128, 256], F32)
    rmat_tr = Tr()
    ones_bf = sb("ones_bf", [128, 128], BF16)
    ident_f = sb("ident_f", [128, 128], F32)
    ident_bf = sb("ident_bf", [128, 128], BF16)
    tri = sb("tri", [128, 128], F32)
    kconst_tr = Tr()
    sgt = [sb(f"sgt{i}", [128, C], F32) for i in range(2)]
    sgt_tr = [Tr() for _ in range(2)]
    rstd_bc = sb("rstd_bc", [128, C], F32)
    rstd_tr = Tr()
    rs_tmp = sb("rs_tmp", [128, C], F32)
    rs_tr = Tr()
    ybase = sb("ybase", [128, C], F32)
    ybase_tr = Tr()
    posi = ybase[:].bitcast(I32)
    posi_tr = ybase_tr
    wabs = sb("wabs", [128, 16, 16], F32)
    wsgn = sb("wsgn", [128, 16, 16], F32)
    wab_tr = [Tr() for _ in range(16)]
    small = sb("small", [128, 64], F32)
    small_tr = [Tr() for _ in range(64)]
    nwt = sb("nwt", [128, 2, NIT + 1], F32)
    nwt_tr = [Tr() for _ in range(2)]
    tot = sb("tot", [128, 2, 128], F32)
    tot_tr = [Tr() for _ in range(2)]

    rl = [sgt[0], sgt[1], rs_tmp, ybase]
    rl_tr = [sgt_tr[0], sgt_tr[1], rs_tr, ybase_tr]
    NPS = 6
    pall = es.enter_context(nc.psum_tensor("pall", [128, 8, C], F32))
    pbank = [pall[:, i, :] for i in range(8)]
    pbank_tr = [Tr() for _ in range(8)]
    pbt = pall[:, 7, :].bitcast(BF16)
    pbt_tr = pbank_tr[7]
    st = {"ps": 0, "ring": 0, "sg": 0, "sg4": 0, "ps2": 0, "ix": 0, "ps3": 0}

    pinned = set()

    def psum(pin=False):
        while True:
            i = st["ps"] % NPS
            st["ps"] += 1
            if i not in pinned:
                break
        if pin:
            pinned.add(i)
        return pbank[i], pbank_tr[i]

    def psum2():
        i0 = 2 * (st["ps2"] % 2)
        st["ps2"] += 1
        return i0

    def unpin(ps):
        for i in range(8):
            if pbank[i] is ps:
                pinned.discard(i)

    def yT_slice(m):
        if m < 12:
            return mixs[:, m, :], [mix_tr[m]]
        i = m - 12
        ap = hT[:, 2 * i:2 * i + 2, :].bitcast(F32).rearrange("p a b -> p (a b)")
        return ap, [hT_tr[2 * i], hT_tr[2 * i + 1]]

    def yT_group(g):
        if g < 3:
            return mixs[:, 4 * g:4 * g + 4, :].rearrange("p a b -> p (a b)"), mix_tr[4 * g:4 * g + 4]
        return hT[:, 0:8, :].bitcast(F32).rearrange("p a b -> p (a b)"), hT_tr[0:8]

    def xres_group(g):
        return xres[:, 4 * g:4 * g + 4, :].rearrange("p a b -> p (a b)"), xres_tr[4 * g:4 * g + 4]

    sq_ap = aT[:, 40:44, :].rearrange("p a b -> p (a b)")
    sq_trs = aT_tr[40:44]
    scores = mixs[:, 0:8, :].rearrange("p a b -> p (a b)")
    scores_tr = mix_tr[0:8]
    scores1 = ringall[:, 1:3, :].bitcast(F32).rearrange("p a b -> p (a b)")
    maskb = mixs[:, 8:12, :].bitcast(BF16).rearrange("p a b -> p (a b)")
    mask_tr = mix_tr[8:12]
    convo = [mixs[:, c, :] for c in range(8)]
    tabs = [mixs[:, 8 + i, :] for i in range(4)]
    tab_tr = mix_tr[8:12]
    maskT = aT[:, 0:8, :].rearrange("p a b -> p (a b)")
    maskT_tr = aT_tr[0:8]
    qiT = [aT[:, 8 + j, :] for j in range(8)]
    qiT_tr = aT_tr[8:16]
    qT = [aT[:, 16 + j, :] for j in range(8)]
    qT_tr = aT_tr[16:24]
    cT = [aT[:, 24 + j, :] for j in range(8)]
    cT_tr = aT_tr[24:32]
    oT = [aT[:, 32 + j, :] for j in range(8)]
    oT_tr = aT_tr[32:40]
    mergedT = [aT[:, j, :] for j in range(16)]
    merged_tr = aT_tr[0:16]

    def xtok(slot):
        ap = aT[:, 8 * slot:8 * slot + 8, :].bitcast(F32).rearrange("p a b -> p (a b)")
        return ap, aT_tr[8 * slot:8 * slot + 8]

    def wtile(W, r0, nk, c0, ncols):
        s = st["ring"] % NR
        st["ring"] += 1
        view = ring[s][:, 0:nk * ncols].rearrange("p (k n) -> p k n", n=ncols)
        src = W[r0 * 128:(r0 + nk) * 128, c0:c0 + ncols].rearrange("(k p) n -> p k n", p=128)
        P.dma("pool", f"w{s}", lambda E, v=view, s_=src: [E.dma_start(out=v, in_=s_)], writes=[ring_tr[s]])
        return view, ring_tr[s]

    def wtile_kiwi():
        s = st["ring"] % NR
        st["ring"] += 1
        view = ring[s][:, 0:16 * 144].rearrange("p (k n) -> p k n", n=144)

        def src(c0, n):
            return Win[:, c0:c0 + n].rearrange("(k p) n -> p k n", p=128)

        def fn(E, v=view):
            return [E.dma_start(out=v[:, :, 0:64], in_=src(4608, 64)),
                    E.dma_start(out=v[:, :, 64:128], in_=src(4608, 64)),
                    E.dma_start(out=v[:, :, 128:144], in_=src(4672, 16))]
        P.dma("pool", f"w{s}", fn, writes=[ring_tr[s]], n=3)
        return view, ring_tr[s]

    def mm_group(ps, ps_tr, pairs, reads, n=C, first=True, last=True):
        def fn(E, ps=ps, pairs=pairs, n=n, first=first, last=last):
            r = None
            for i, (l, rh) in enumerate(pairs):
                r = E.matmul(ps[:, 0:n], lhsT=l, rhs=rh, start=(first and i == 0),
                             stop=(last and i == len(pairs) - 1))
            return r
        P.op("pe", fn, reads=reads, writes=[ps_tr])

    def evac(i, out_ap, in_ap, reads, writes):
        if i % 2 == 0:
            P.op("act", lambda E, o=out_ap, a=in_ap: E.copy(out=o, in_=a), reads=reads, writes=writes)
        else:
            P.op("dve", lambda E, o=out_ap, a=in_ap: E.tensor_copy(out=o, in_=a), reads=reads, writes=writes)

    def rms_rstd(group_fn, ngroups, dim):
        ps, ps_tr = psum()
        for g in range(ngroups):
            src, trs = group_fn(g)
            P.op("act", lambda E, s=src: E.activation(out=sq_ap, in_=s, func=AF.Square),
                 reads=trs, writes=sq_trs)
            pairs = [(ones_bf[:], aT[:, 40 + kk, :]) for kk in range(4)]
            mm_group(ps, ps_tr, pairs, reads=list(sq_trs) + [kconst_tr], first=(g == 0), last=(g == ngroups - 1))
        P.op("act", lambda E, ps=ps: E.activation(out=rs_tmp[:], in_=ps[:], func=AF.Sqrt, scale=1.0 / dim,
                                                   bias=small[:, 63:64]),
             reads=[ps_tr, small_tr[63]], writes=[rs_tr])
        P.op("dve", lambda E: E.reciprocal(out=rstd_bc[:], in_=rs_tmp[:]), reads=[rs_tr], writes=[rstd_tr])

    def norm_apply(gcol):
        for k in range(16):
            P.op("dve", lambda E, k=k: E.scalar_tensor_tensor(
                out=hT[:, k, :], in0=xres[:, k, :], scalar=cst[:, gcol + k:gcol + k + 1],
                in1=rstd_bc[:], op0=ALU.mult, op1=ALU.mult),
                reads=[xres_tr[k], rstd_tr, cst_tr], writes=[hT_tr[k]])

    def residual_update(gtile, gcol):
        for k in range(16):
            yap, ytrs = yT_slice(k)
            i = st["sg"] % 2
            st["sg"] += 1
            P.op("dve", lambda E, k=k, yap=yap, i=i: E.scalar_tensor_tensor(
                out=sgt[i][:], in0=yap, scalar=gtile[:, gcol + k:gcol + k + 1], in1=rstd_bc[:],
                op0=ALU.mult, op1=ALU.mult),
                reads=list(ytrs) + [rstd_tr, cst_tr, gh_tr], writes=[sgt_tr[i]])
            P.op("dve", lambda E, k=k, i=i: E.tensor_tensor(out=xres[:, k, :], in0=xres[:, k, :], in1=sgt[i][:],
                                                             op=ALU.add),
                 reads=[sgt_tr[i], xres_tr[k]], writes=[xres_tr[k]])

    def ffn(W1, W2, gpre, gpost_half_col):
        rms_rstd(xres_group, 4, D)
        norm_apply(gpre)
        hreads = list(hT_tr)
        for f2 in range(NFF // 2):
            Wg, Wg_tr = wtile(W1, 0, 16, f2 * 256, 256)
            Wu, Wu_tr = wtile(W1, 0, 16, DFF + f2 * 256, 256)
            pgs = []
            for j in range(2):
                pg, pg_tr = psum()
                mm_group(pg, pg_tr, [(Wg[:, k, j * 128:(j + 1) * 128], hT[:, k, :]) for k in range(16)],
                         reads=hreads + [Wg_tr])
                pgs.append((pg, pg_tr))
            for j in range(2):
                f = 2 * f2 + j
                pg, pg_tr = pgs[j]
                pu, pu_tr = psum()
                mm_group(pu, pu_tr, [(Wu[:, k, j * 128:(j + 1) * 128], hT[:, k, :]) for k in range(16)],
                         reads=hreads + [Wu_tr])
                i = st["sg"] % 2
                st["sg"] += 1
                P.op("act", lambda E, pg=pg, i=i: E.activation(out=sgt[i][:], in_=pg[:], func=AF.Silu),
                     reads=[pg_tr], writes=[sgt_tr[i]])
                P.op("dve", lambda E, pu=pu, i=i, f=f: E.tensor_tensor(out=aT[:, f, :], in0=sgt[i][:], in1=pu[:],
                                                                       op=ALU.mult),
                     reads=[sgt_tr[i], pu_tr], writes=[aT_tr[f]])
        for m2 in range(8):
            pa, pa_tr = psum()
            pb_, pb_tr = psum()
            for kq in range(4):
                Wt, Wt_tr = wtile(W2, kq * 11, 11, m2 * 256, 256)
                rd = aT_tr[kq * 11:(kq + 1) * 11] + [Wt_tr]
                mm_group(pa, pa_tr, [(Wt[:, kk, 0:128], aT[:, kq * 11 + kk, :]) for kk in range(11)], reads=rd,
                         first=(kq == 0), last=(kq == 3))
                mm_group(pb_, pb_tr, [(Wt[:, kk, 128:256], aT[:, kq * 11 + kk, :]) for kk in range(11)], reads=rd,
                         first=(kq == 0), last=(kq == 3))
            ya, ya_trs = yT_slice(2 * m2)
            yb, yb_trs = yT_slice(2 * m2 + 1)
            evac(0, ya, pa[:], [pa_tr], ya_trs)
            evac(1, yb, pb_[:], [pb_tr], yb_trs)
        rms_rstd(yT_group, 4, D)
        residual_update(gh, gpost_half_col)

    def load_chunk(cc):
        for tt in range(4):
            slot = (cc * 4 + tt) % 2
            xt, xt_trs = xtok(slot)
            row0 = (cc * 4 + tt) * 128
            P.dma("sp", f"x{slot}", lambda E, xt=xt, row0=row0: [E.dma_start(out=xt, in_=xin[row0:row0 + 128, :])],
                  writes=xt_trs)
            for kg in range(4):
                ps, ps_tr = psum()

                def fn(E, ps=ps, xt=xt, kg=kg):
                    r = None
                    for kk in range(4):
                        r = E.transpose(out=ps[:, kk * 128:(kk + 1) * 128],
                                        in_=xt[:, (kg * 4 + kk) * 128:(kg * 4 + kk + 1) * 128], identity=ident_f[:])
                    return r
                P.op("pe", fn, reads=list(xt_trs) + [kconst_tr], writes=[ps_tr])
                evac(kg, xres[:, kg * 4:kg * 4 + 4, tt * 128:(tt + 1) * 128],
                     ps[:].rearrange("p (a b) -> p a b", a=4), [ps_tr], xres_tr[kg * 4:kg * 4 + 4])

    def store_chunk(c):
        for tt in range(4):
            slot = tt % 2
            xt, xt_trs = xtok(slot)
            for kg in range(4):
                ps, ps_tr = psum()

                def fn(E, ps=ps, kg=kg, tt=tt):
                    r = None
                    for kk in range(4):
                        r = E.transpose(out=ps[:, kk * 128:(kk + 1) * 128],
                                        in_=xres[:, kg * 4 + kk, tt * 128:(tt + 1) * 128], identity=ident_f[:])
                    return r
                P.op("pe", fn, reads=xres_tr[kg * 4:kg * 4 + 4] + [kconst_tr], writes=[ps_tr])
                evac(kg, xt[:, kg * 512:(kg + 1) * 512], ps[:], [ps_tr], xt_trs[2 * kg:2 * kg + 2])
            row0 = (c * 4 + tt) * 128
            P.dma("sp", f"o{slot}", lambda E, xt=xt, row0=row0: [E.dma_start(out=outd[row0:row0 + 128, :], in_=xt)],
                  reads=xt_trs)

    def make_tables(cc):
        P.dma("sp", "pos", lambda E, cc=cc: [E.dma_start(out=posi, in_=posb[:, cc * C:(cc + 1) * C])],
              writes=[posi_tr])
        P.op("dve", lambda E: E.tensor_copy(out=rs_tmp[:], in_=posi), reads=[posi_tr], writes=[rs_tr])
        for typ in range(2):
            P.op("dve", lambda E, typ=typ: E.tensor_scalar(out=ybase[:], in0=rs_tmp[:],
                                                          scalar1=cst[:, C_INVF + typ:C_INVF + typ + 1],
                                                          scalar2=None, op0=ALU.mult),
                 reads=[rs_tr, cst_tr], writes=[ybase_tr])
            for cs in range(2):
                dst = tabs[2 * typ + cs]
                dtr = [tab_tr[2 * typ + cs]]
                i = st["sg"] % 2
                st["sg"] += 1
                shift = 0.25 if cs == 0 else 0.0
                P.op("dve", lambda E, i=i, shift=shift: E.tensor_scalar(out=sgt[i][:], in0=ybase[:], scalar1=shift,
                                                                       scalar2=None, op0=ALU.add),
                     reads=[ybase_tr], writes=[sgt_tr[i]])
                P.op("dve", lambda E, i=i, dst=dst: E.tensor_scalar(out=dst, in0=sgt[i][:], scalar1=MAGIC,
                                                                   scalar2=MAGIC, op0=ALU.add, op1=ALU.subtract),
                     reads=[sgt_tr[i]], writes=dtr)
                P.op("dve", lambda E, i=i, dst=dst: E.tensor_tensor(out=dst, in0=sgt[i][:], in1=dst, op=ALU.subtract),
                     reads=[sgt_tr[i]] + dtr, writes=dtr)
                P.op("dve", lambda E, dst=dst: E.tensor_scalar(out=dst, in0=dst, scalar1=0.4999995,
                                                              scalar2=-0.4999995, op0=ALU.min, op1=ALU.max),
                     reads=dtr, writes=dtr)
                P.op("act", lambda E, dst=dst: E.activation(out=dst, in_=dst, func=AF.Sin, scale=TWO_PI),
                     reads=dtr, writes=dtr)

    def rope(ps, ps_tr, typ, dst_ap, dst_trs):
        i = st["sg"] % 2
        st["sg"] += 1
        P.op("act", lambda E, ps=ps, i=i: E.copy(out=sgt[i][:], in_=ps[:]), reads=[ps_tr], writes=[sgt_tr[i]])
        pr, pr_tr = psum()
        mm_group(pr, pr_tr, [(rmat[:, typ * 128:(typ + 1) * 128], sgt[i][:])], reads=[sgt_tr[i], rmat_tr])
        P.op("dve", lambda E, pr=pr, typ=typ: E.tensor_tensor(out=rs_tmp[:], in0=pr[:], in1=tabs[2 * typ + 1],
                                                             op=ALU.mult),
             reads=[pr_tr, tab_tr[2 * typ + 1]], writes=[rs_tr])
        P.op("dve", lambda E, i=i, typ=typ: E.tensor_tensor(out=sgt[i][:], in0=sgt[i][:], in1=tabs[2 * typ],
                                                           op=ALU.mult),
             reads=[sgt_tr[i], tab_tr[2 * typ]], writes=[sgt_tr[i]])
        P.op("dve", lambda E, i=i, dst_ap=dst_ap: E.tensor_tensor(out=dst_ap, in0=sgt[i][:], in1=rs_tmp[:],
                                                                 op=ALU.add),
             reads=[sgt_tr[i], rs_tr], writes=dst_trs)

    def proj_group(c0, ncols):
        Wt, Wt_tr = wtile(Win, 0, 16, c0, ncols)
        return Wt, Wt_tr

    def proj_ps(Wt, Wt_tr, j, n=C, tok0=0):
        ps, ps_tr = psum()
        mm_group(ps, ps_tr, [(Wt[:, k, j * 128:(j + 1) * 128], hT[:, k, tok0:tok0 + n]) for k in range(16)],
                 reads=list(hT_tr) + [Wt_tr], n=n)
        return ps, ps_tr

    def kv_proj(cc):
        k0 = cc * C
        Wt, Wt_tr = proj_group(3072, 256)
        for g in range(2):
            ps, ps_tr = proj_ps(Wt, Wt_tr, g)
            rope(ps, ps_tr, 0, Kc[:, g, k0:k0 + C], [Kc_tr])
        Wt, Wt_tr = proj_group(3328, 256)
        for tt in range(4):
            ps, ps_tr = psum()
            mm_group(ps, ps_tr, [(hT[:, k, tt * 128:(tt + 1) * 128], Wt[:, k, :]) for k in range(16)],
                     reads=list(hT_tr) + [Wt_tr], n=256)
            evac(tt, Vc[:, cc * 4 + tt, :], ps[:, 0:256], [ps_tr], [Vc_tr])
        Wk, Wk_tr = wtile_kiwi()
        ps, ps_tr = proj_ps(Wk, Wk_tr, 0)
        rope(ps, ps_tr, 1, Kic[:, k0:k0 + C], [Kic_tr])
        return Wk, Wk_tr

    def wi_proj(c, Wk, Wk_tr):
        for r in range(4):
            i = 4 * c + r
            ps, ps_tr = psum()
            mm_group(ps, ps_tr, [(hT[:, k, r * 128:(r + 1) * 128], Wk[:, k, 128:144]) for k in range(16)],
                     reads=list(hT_tr) + [Wk_tr], n=16)
            P.op("act", lambda E, ps=ps, i=i: E.copy(out=wabs[:, i, :], in_=ps[:, 0:16]),
                 reads=[ps_tr], writes=[wab_tr[i]])

    def conv_ab(n, tok0, dst_off):
        for grp in range(4):
            Wt, Wt_tr = proj_group(1024 + grp * 256, 256)
            for j in range(2):
                c = 2 * grp + j
                ps, ps_tr = proj_ps(Wt, Wt_tr, j, n=n, tok0=tok0)
                P.op("act", lambda E, ps=ps, c=c: E.activation(out=ubuf[:, c, dst_off:dst_off + n], in_=ps[:, 0:n],
                                                               func=AF.Sigmoid),
                     reads=[ps_tr], writes=[ub_tr[c]])
        for grp in range(4):
            Wt, Wt_tr = proj_group(grp * 256, 256)
            for j in range(2):
                c = 2 * grp + j
                ps, ps_tr = proj_ps(Wt, Wt_tr, j, n=n, tok0=tok0)
                P.op("dve", lambda E, ps=ps, c=c: E.tensor_tensor(out=ubuf[:, c, dst_off:dst_off + n],
                                                                  in0=ubuf[:, c, dst_off:dst_off + n],
                                                                  in1=ps[:, 0:n], op=ALU.mult),
                     reads=[ps_tr, ub_tr[c]], writes=[ub_tr[c]])

    diag = aT[:, 40:44, :].rearrange("p a b -> p (a b)").rearrange("p (t n) -> p t n", n=128)

    def conv_branch():
        for c in range(8):
            pc_, pc_tr = psum(pin=True)
            for g in range(8):
                taps = list(range(4 * g, min(4 * g + 4, 31)))
                dtr = aT_tr[40 + g % 4]
                for jj, j in enumerate(taps):
                    P.op("dve", lambda E, c=c, j=j, t=(g % 4) * 4 + jj: E.tensor_scalar(
                        out=diag[:, t, :], in0=ident_bf[:], scalar1=cst[:, C_DW + c * 31 + j:C_DW + c * 31 + j + 1],
                        scalar2=None, op0=ALU.mult),
                        reads=[kconst_tr, cst_tr], writes=[dtr])
                mm_group(pc_, pc_tr, [(diag[:, (g % 4) * 4 + jj, :], ubuf[:, c, 2 + j:2 + j + C])
                                      for jj, j in enumerate(taps)],
                         reads=[dtr, ub_tr[c]], first=(g == 0), last=(g == 7))
            unpin(pc_)
            P.op("act", lambda E, pc_=pc_, c=c: E.activation(out=cT[c], in_=pc_[:], func=AF.Identity,
                                                              bias=cst[:, C_DWB + c:C_DWB + c + 1]),
                 reads=[pc_tr, cst_tr], writes=[cT_tr[c]])
        for c in range(8):
            P.op("dve", lambda E, c=c: E.tensor_copy(out=ubuf[:, c, 0:32], in_=ubuf[:, c, C:C + 32]),
                 reads=[ub_tr[c]], writes=[ub_tr[c]])
        pm, pm_tr = psum(pin=True)
        pq, pq_tr = psum(pin=True)
        for g in range(2):
            src = aT[:, 24 + 4 * g:24 + 4 * g + 4, :].rearrange("p a b -> p (a b)")
            mm_group(pm, pm_tr, [(ones_bf[:], cT[4 * g + kk]) for kk in range(4)],
                     reads=list(cT_tr[4 * g:4 * g + 4]) + [kconst_tr], first=(g == 0), last=(g == 1))
            P.op("act", lambda E, src=src: E.activation(out=sq_ap, in_=src, func=AF.Square),
                 reads=cT_tr[4 * g:4 * g + 4], writes=sq_trs)
            mm_group(pq, pq_tr, [(ones_bf[:], aT[:, 40 + kk, :]) for kk in range(4)],
                     reads=list(sq_trs) + [kconst_tr], first=(g == 0), last=(g == 1))
        unpin(pm)
        unpin(pq)
        P.op("act", lambda E, pm=pm: E.activation(out=ybase[:], in_=pm[:], func=AF.Copy, scale=1.0 / 1024),
             reads=[pm_tr], writes=[ybase_tr])
        P.op("dve", lambda E: E.tensor_tensor(out=rs_tmp[:], in0=ybase[:], in1=ybase[:], op=ALU.mult),
             reads=[ybase_tr], writes=[rs_tr])
        P.op("dve", lambda E, pq=pq: E.scalar_tensor_tensor(out=rs_tmp[:], in0=pq[:], scalar=1.0 / 1024,
                                                           in1=rs_tmp[:], op0=ALU.mult, op1=ALU.subtract),
             reads=[pq_tr, rs_tr], writes=[rs_tr])
        P.op("act", lambda E: E.activation(out=rs_tmp[:], in_=rs_tmp[:], func=AF.Sqrt, bias=small[:, 63:64]),
             reads=[rs_tr, small_tr[63]], writes=[rs_tr])
        P.op("dve", lambda E: E.reciprocal(out=rstd_bc[:], in_=rs_tmp[:]), reads=[rs_tr], writes=[rstd_tr])
        for c in range(8):
            i = st["sg"] % 2
            st["sg"] += 1
            P.op("dve", lambda E, c=c, i=i: E.tensor_tensor(out=sgt[i][:], in0=cT[c], in1=ybase[:], op=ALU.subtract),
                 reads=[cT_tr[c], ybase_tr], writes=[sgt_tr[i]])
            P.op("dve", lambda E, i=i: E.tensor_tensor(out=sgt[i][:], in0=sgt[i][:], in1=rstd_bc[:], op=ALU.mult),
                 reads=[sgt_tr[i], rstd_tr], writes=[sgt_tr[i]])
            P.op("act", lambda E, c=c, i=i: E.activation(out=cT[c], in_=sgt[i][:], func=AF.Silu,
                                                         scale=cst[:, C_LNG + c:C_LNG + c + 1],
                                                         bias=cst[:, C_LNB + c:C_LNB + c + 1]),
                 reads=[sgt_tr[i], cst_tr], writes=[cT_tr[c]])

    class Tile:
        def __init__(self, c, r):
            self.c, self.r = c, r
            self.i = 4 * c + r
            self.qb = NPRE * C + 128 * self.i
            self.NK = self.qb + 128
            self.nblk = (self.NK + 511) // 512
            self.nkt = self.NK // 128
            self.pp = self.i % 2
            if self.pp == 0:
                self.sc = scores
                self.btr = lambda b: scores_tr[b]
            else:
                self.sc = scores1
                self.btr = lambda b: ring_tr[1 + b // 4]
            self.ntr = list({id(self.btr(b)): self.btr(b) for b in range(self.nblk)}.values())

        def sm(self, j):
            return small[:, self.pp * 24 + j:self.pp * 24 + j + 1]

        def smt(self, j):
            return small_tr[self.pp * 24 + j]

    ixb = [rs_tmp, ybase]
    ixb_tr = [rs_tr, ybase_tr]

    def indexer_gen(T):
        i, r, NK, nblk, qb = T.i, T.r, T.NK, T.nblk, T.qb
        sm, smt, sc = T.sm, T.smt, T.sc
        for h in range(16):
            P.op("dve", lambda E, h=h: E.tensor_scalar(out=diag[:, h, :], in0=ident_bf[:],
                                                      scalar1=wabs[:, i, h:h + 1], scalar2=None, op0=ALU.mult),
                 reads=[kconst_tr, wab_tr[i]], writes=[aT_tr[40 + h // 4]])
        for b in range(nblk):
            wd = min(512, NK - 512 * b)
            pacc, pacc_tr = pbank[6], pbank_tr[6]

            def emit_dots(jp, b=b, wd=wd):
                i0 = psum2()

                def fn(E, i0=i0, jp=jp, b=b, wd=wd):
                    E.matmul(pbank[i0][:, 0:wd], lhsT=qiT[jp][0:64, r * 128:(r + 1) * 128],
                             rhs=Kic[0:64, 512 * b:512 * b + wd], start=True, stop=True)
                    return E.matmul(pbank[i0 + 1][:, 0:wd], lhsT=qiT[jp][64:128, r * 128:(r + 1) * 128],
                                    rhs=Kic[64:128, 512 * b:512 * b + wd], start=True, stop=True)
                P.op("pe", fn, reads=[qiT_tr[jp], Kic_tr], writes=[pbank_tr[i0], pbank_tr[i0 + 1]])
                return i0
            nxt = emit_dots(0)
            for jp in range(8):
                i0 = nxt
                if jp + 1 < 8:
                    nxt = emit_dots(jp + 1)
                k = st["ix"] % 2
                st["ix"] += 1
                rlb = ixb[k][:].bitcast(BF16).rearrange("p (a b) -> p a b", a=2)
                P.op("act", lambda E, i0=i0, rlb=rlb, wd=wd: E.activation(
                    out=rlb[:, :, 0:wd], in_=pall[:, i0:i0 + 2, 0:wd], func=AF.Relu),
                    reads=[pbank_tr[i0], pbank_tr[i0 + 1]], writes=[ixb_tr[k]])

                def fn2(E, jp=jp, rlb=rlb, wd=wd, pacc=pacc):
                    E.matmul(pacc[:, 0:wd], lhsT=diag[:, 2 * jp, :], rhs=rlb[:, 0, 0:wd], start=(jp == 0), stop=False)
                    return E.matmul(pacc[:, 0:wd], lhsT=diag[:, 2 * jp + 1, :], rhs=rlb[:, 1, 0:wd], start=False,
                                    stop=(jp == 7))
                P.op("pe", fn2, reads=[ixb_tr[k], aT_tr[40 + (2 * jp) // 4]], writes=[pacc_tr])
                if jp == 7:
                    P.op("dve", lambda E, b=b, wd=wd, pacc=pacc: E.tensor_copy(out=sc[:, 512 * b:512 * b + wd],
                                                                              in_=pacc[:, 0:wd]),
                         reads=[pacc_tr], writes=[T.btr(b)])
                yield
        ntr = T.ntr
        P.op("dve", lambda E: E.tensor_reduce(out=sm(0), in_=sc[:, 0:NK], axis=AX.X, op=ALU.max,
                                              apply_absolute_value=True),
             reads=ntr, writes=[smt(0)])
        P.op("dve", lambda E: E.tensor_scalar(out=sm(1), in0=sm(0), scalar1=1.001, scalar2=1e-30, op0=ALU.mult,
                                              op1=ALU.add),
             reads=[smt(0)], writes=[smt(1)])
        P.op("dve", lambda E: E.tensor_scalar(out=nwt[:, T.pp, :], in0=cst[:, C_BIS:C_BIS + NIT + 1], scalar1=sm(1),
                                              scalar2=None, op0=ALU.mult),
             reads=[smt(1), cst_tr], writes=[nwt_tr[T.pp]])
        ptr = list({id(T.btr(b)): T.btr(b) for b in range(4)}.values())
        P.op("dve", lambda E: E.tensor_scalar(out=sc[:, 0:NPRE * C], in0=sc[:, 0:NPRE * C],
                                              scalar1=cst[:, C_VFLAG:C_VFLAG + 1],
                                              scalar2=cst[:, C_NEGBIG:C_NEGBIG + 1], op0=ALU.mult, op1=ALU.add),
             reads=ptr + [cst_tr], writes=ptr)
        dtr = [T.btr(qb // 512)]
        P.op("dve", lambda E: E.tensor_tensor(out=sc[:, qb:qb + 128], in0=sc[:, qb:qb + 128], in1=tri[:],
                                              op=ALU.add),
             reads=dtr + [kconst_tr], writes=dtr)
        P.op("dve", lambda E: E.memset(sm(2), 0.0), writes=[smt(2)])
        cthr = -(512.0 - NK) + 0.5
        P.op("dve", lambda E: E.memset(sm(6), cthr), writes=[smt(6)])
        yield

    def bisect_gen(T):
        NK, sc, ntr = T.NK, T.sc, T.ntr
        sm, smt = T.sm, T.smt
        for it in range(NIT):
            cur, nxt = 2 + (it % 2), 2 + ((it + 1) % 2)
            if it % 4 == 0:
                P.op("act", lambda E, cur=cur: E.activation(out=maskb[:, 0:NK], in_=sc[:, 0:NK], func=AF.Sign,
                                                            bias=sm(cur), accum_out=sm(4)),
                     reads=list(ntr) + [smt(cur)], writes=list(mask_tr) + [smt(4)])
                P.op("act", lambda E: E.activation(out=sm(5), in_=sm(4), func=AF.Sign, bias=sm(6)),
                     reads=[smt(4), smt(6)], writes=[smt(5)])
            else:
                P.op("dve", lambda E, cur=cur: E.tensor_scalar(out=sm(8), in0=sm(cur), scalar1=-1.0,
                                                              scalar2=None, op0=ALU.mult),
                     reads=[smt(cur)], writes=[smt(8)])
                P.op("dve", lambda E: E.tensor_scalar(out=maskb[:, 0:NK], in0=sc[:, 0:NK],
                                                      scalar1=sm(8), scalar2=0.0, op0=ALU.is_ge,
                                                      op1=ALU.add, accum_out=sm(4)),
                     reads=list(ntr) + [smt(8)], writes=list(mask_tr) + [smt(4)])
                P.op("dve", lambda E: E.tensor_scalar(out=sm(5), in0=sm(4), scalar1=255.5, scalar2=2.0,
                                                      op0=ALU.is_ge, op1=ALU.mult),
                     reads=[smt(4)], writes=[smt(5)])
                P.op("dve", lambda E: E.tensor_scalar(out=sm(5), in0=sm(5), scalar1=-1.0, scalar2=None,
                                                      op0=ALU.add),
                     reads=[smt(5)], writes=[smt(5)])
            if it % 4 == 0:
                P.op("act", lambda E, cur=cur, nxt=nxt, it=it: E.activation(
                    out=sm(nxt), in_=sm(5), func=AF.Identity, scale=nwt[:, T.pp, it + 1:it + 2], bias=sm(cur)),
                    reads=[smt(5), smt(cur), nwt_tr[T.pp]], writes=[smt(nxt)])
            else:
                P.op("dve", lambda E, cur=cur, nxt=nxt, it=it: E.scalar_tensor_tensor(
                    out=sm(nxt), in0=sm(5), scalar=nwt[:, T.pp, it + 1:it + 2], in1=sm(cur),
                    op0=ALU.mult, op1=ALU.add),
                    reads=[smt(5), smt(cur), nwt_tr[T.pp]], writes=[smt(nxt)])
            yield

    def mask_and_T(T):
        NK, nkt, sc, ntr = T.NK, T.nkt, T.sc, T.ntr
        sm, smt = T.sm, T.smt
        fin = 2 + (NIT % 2)
        P.op("dve", lambda E: E.tensor_scalar(out=sm(7), in0=sm(fin), scalar1=-1.0,
                                              scalar2=nwt[:, T.pp, NIT:NIT + 1], op0=ALU.mult, op1=ALU.add),
             reads=[smt(fin), nwt_tr[T.pp]], writes=[smt(7)])
        P.op("dve", lambda E: E.tensor_scalar(out=maskb[:, 0:NK], in0=sc[:, 0:NK], scalar1=sm(7), scalar2=None,
                                              op0=ALU.is_ge),
             reads=list(ntr) + [smt(7)], writes=mask_tr)
        for j0 in range(0, nkt, 8):
            nj = min(8, nkt - j0)

            def fn(E, j0=j0, nj=nj):
                r_ = None
                for jj in range(nj):
                    r_ = E.transpose(out=pbt[:, jj * 128:(jj + 1) * 128],
                                     in_=maskb[:, (j0 + jj) * 128:(j0 + jj + 1) * 128], identity=ident_bf[:])
                return r_
            P.op("pe", fn, reads=list(mask_tr) + [kconst_tr], writes=[pbt_tr])
            evac(j0 // 8, maskT[:, j0 * 128:(j0 + nj) * 128], pbt[:, 0:nj * 128], [pbt_tr], maskT_tr)

    def attention_gen(T):
        r, nkt = T.r, T.nkt
        ngrp = (nkt + 3) // 4
        steps = [(h, kg) for h in range(8) for kg in range(ngrp)]
        pacc7, pacc7_tr = pbank[7], pbank_tr[7]

        def emit_st(h, kg):
            g = h // 4
            j0 = 4 * kg
            nj = min(4, nkt - j0)
            ib = 4 + (st["ps3"] % 2)
            st["ps3"] += 1
            pS, pS_tr = pbank[ib], pbank_tr[ib]

            def fn(E, pS=pS, j0=j0, nj=nj, h=h, g=g):
                r_ = None
                for jj in range(nj):
                    r_ = E.matmul(pS[:, jj * 128:(jj + 1) * 128],
                                  lhsT=Kc[:, g, (j0 + jj) * 128:(j0 + jj + 1) * 128],
                                  rhs=qT[h][:, r * 128:(r + 1) * 128], start=True, stop=True)
                return r_
            P.op("pe", fn, reads=[Kc_tr, qT_tr[h]], writes=[pS_tr])
            return pS, pS_tr

        nxt = emit_st(*steps[0])
        for si, (h, kg) in enumerate(steps):
            g = h // 4
            pS, pS_tr = nxt
            if si + 1 < len(steps):
                nxt = emit_st(*steps[si + 1])
            j0 = 4 * kg
            nj = min(4, nkt - j0)
            wd = nj * 128
            k = st["sg"] % 2
            st["sg"] += 1
            pt = sgt[k][:].bitcast(BF16)
            P.op("act", lambda E, pS=pS, pt=pt, wd=wd: E.activation(out=pt[:, 0:wd], in_=pS[:, 0:wd], func=AF.Exp,
                                                                    scale=ATT_SCALE),
                 reads=[pS_tr], writes=[sgt_tr[k]])
            P.op("dve", lambda E, pt=pt, wd=wd, j0=j0: E.tensor_tensor(
                out=pt[:, 512:512 + wd], in0=pt[:, 0:wd], in1=maskT[:, j0 * 128:j0 * 128 + wd], op=ALU.mult),
                reads=[sgt_tr[k]] + list(maskT_tr), writes=[sgt_tr[k]])

            def fn2(E, pt=pt, j0=j0, nj=nj, g=g, kg=kg):
                r_ = None
                for jj in range(nj):
                    E.matmul(pacc7[:, 0:128], lhsT=Vc[:, j0 + jj, g * 128:(g + 1) * 128],
                             rhs=pt[:, 512 + jj * 128:512 + (jj + 1) * 128],
                             start=(kg == 0 and jj == 0), stop=False, skip_group_check=True)
                    r_ = E.matmul(pacc7[:, 128:256], lhsT=ones_bf[:], rhs=pt[:, 512 + jj * 128:512 + (jj + 1) * 128],
                                  start=False, stop=(kg == ngrp - 1 and jj == nj - 1), skip_group_check=True)
                return r_
            P.op("pe", fn2, reads=[Vc_tr, sgt_tr[k], kconst_tr], writes=[pacc7_tr])
            if kg == ngrp - 1:
                tk = h % 2
                P.op("dve", lambda E, tk=tk: E.reciprocal(out=tot[:, tk, :], in_=pacc7[:, 128:256]),
                     reads=[pacc7_tr], writes=[tot_tr[tk]])
                P.op("dve", lambda E, tk=tk, h=h: E.tensor_tensor(out=oT[h][:, r * 128:(r + 1) * 128],
                                                                 in0=pacc7[:, 0:128], in1=tot[:, tk, :],
                                                                 op=ALU.mult),
                     reads=[pacc7_tr, tot_tr[tk]], writes=[oT_tr[h]])
            yield

    def run_streams(streams):
        done = [0] * len(streams)
        alive = set(range(len(streams)))
        while alive:
            j = min(alive, key=lambda j: (done[j] + 1) / float(streams[j][1]))
            try:
                next(streams[j][0])
                done[j] += 1
            except StopIteration:
                alive.discard(j)

    def attn_chunk(c):
        tiles = [Tile(c, r) for r in range(4)]
        run_streams([(indexer_gen(tiles[0]), 8 * tiles[0].nblk + 1)])
        for r in range(4):
            T = tiles[r]
            streams = [(bisect_gen(T), NIT)]
            if r + 1 < 4:
                streams.append((indexer_gen(tiles[r + 1]), 8 * tiles[r + 1].nblk + 1))
            if r >= 1:
                streams.append((attention_gen(tiles[r - 1]), 8 * ((tiles[r - 1].nkt + 3) // 4)))
            run_streams(streams)
            mask_and_T(T)
        run_streams([(attention_gen(tiles[3]), 8 * ((tiles[3].nkt + 3) // 4))])

    def merge_and_out():
        for m2 in range(8):
            Wp, Wp_tr = wtile(Wpw, 0, 8, m2 * 256, 256)
            Wa, Wa_tr = wtile(Wo, 0, 8, m2 * 256, 256)
            for j in range(2):
                pyc, pyc_tr = psum()
                pya, pya_tr = psum()
                mm_group(pyc, pyc_tr, [(Wp[:, k, j * 128:(j + 1) * 128], cT[k]) for k in range(8)],
                         reads=list(cT_tr) + [Wp_tr])
                mm_group(pya, pya_tr, [(Wa[:, k, j * 128:(j + 1) * 128], oT[k]) for k in range(8)],
                         reads=list(oT_tr) + [Wa_tr])
                st.setdefault("ycya", []).append((pyc, pyc_tr, pya, pya_tr))
            Wg0, Wg0_tr = wtile(Win, 0, 16, 4688 + m2 * 256, 256)
            Wg1, Wg1_tr = wtile(Win, 0, 16, 4688 + 2048 + m2 * 256, 256)
            for j in range(2):
                m = 2 * m2 + j
                pyc, pyc_tr, pya, pya_tr = st["ycya"].pop(0)
                pg0, pg0_tr = proj_ps(Wg0, Wg0_tr, j)
                k0 = st["sg"] % 2
                st["sg"] += 1
                P.op("act", lambda E, pg0=pg0, k0=k0: E.activation(out=sgt[k0][:], in_=pg0[:], func=AF.Sigmoid),
                     reads=[pg0_tr], writes=[sgt_tr[k0]])
                P.op("dve", lambda E, pyc=pyc, k0=k0: E.tensor_tensor(out=sgt[k0][:], in0=sgt[k0][:], in1=pyc[:],
                                                                     op=ALU.mult),
                     reads=[sgt_tr[k0], pyc_tr], writes=[sgt_tr[k0]])
                pg1, pg1_tr = proj_ps(Wg1, Wg1_tr, j)
                P.op("act", lambda E, pg1=pg1: E.activation(out=rs_tmp[:], in_=pg1[:], func=AF.Sigmoid),
                     reads=[pg1_tr], writes=[rs_tr])
                P.op("dve", lambda E, pya=pya: E.tensor_tensor(out=rs_tmp[:], in0=rs_tmp[:], in1=pya[:], op=ALU.mult),
                     reads=[rs_tr, pya_tr], writes=[rs_tr])
                P.op("dve", lambda E, k0=k0, m=m: E.tensor_tensor(out=mergedT[m], in0=sgt[k0][:], in1=rs_tmp[:],
                                                                 op=ALU.add),
                     reads=[sgt_tr[k0], rs_tr], writes=[merged_tr[m]])
        for m2 in range(8):
            Wt, Wt_tr = wtile(Wout, 0, 16, m2 * 256, 256)
            for j in range(2):
                m = 2 * m2 + j
                ps, ps_tr = psum()
                mm_group(ps, ps_tr, [(Wt[:, k, j * 128:(j + 1) * 128], mergedT[k]) for k in range(16)],
                         reads=list(merged_tr) + [Wt_tr])
                ya, ya_trs = yT_slice(m)
                evac(m, ya, ps[:], [ps_tr], ya_trs)
        rms_rstd(yT_group, 4, D)
        residual_update(cst, G_MIXPOST)

    P.dma("sp", "c0", lambda E: [E.dma_start(out=cst[:], in_=cstd), E.dma_start(out=rmat[:], in_=rmatd)],
          writes=[cst_tr, rmat_tr], n=2)

    ktrs = {n: Tr() for n in ("ones", "identf", "identb", "tri")}
    P.op("pool", lambda E: E.memset(ones_bf[:], 1.0), writes=[ktrs["ones"]])
    P.op("pool", lambda E: E.memset(ident_f[:], 0.0), writes=[ktrs["identf"]])
    P.op("pool", lambda E: E.affine_select(out=ident_f[:], in_=ident_f[:], pattern=[[-1, 128]],
                                           compare_op=ALU.not_equal, fill=1.0, base=0, channel_multiplier=1),
         reads=[ktrs["identf"]], writes=[ktrs["identf"]])
    P.op("pool", lambda E: E.memset(ident_bf[:], 0.0), writes=[ktrs["identb"]])
    P.op("pool", lambda E: E.affine_select(out=ident_bf[:], in_=ident_bf[:], pattern=[[-1, 128]],
                                           compare_op=ALU.not_equal, fill=1.0, base=0, channel_multiplier=1),
         reads=[ktrs["identb"]], writes=[ktrs["identb"]])
    P.op("pool", lambda E: E.memset(tri[:], 0.0), writes=[ktrs["tri"]])
    P.op("pool", lambda E: E.affine_select(out=tri[:], in_=tri[:], pattern=[[-1, 128]], compare_op=ALU.is_ge,
                                           fill=-1e30, base=0, channel_multiplier=1),
         reads=[ktrs["tri"]], writes=[ktrs["tri"]])
    P.op("pool", lambda E: E.memset(small[:], 0.0), writes=small_tr)
    P.op("pool", lambda E: E.memset(small[:, 63:64], EPS), reads=[small_tr[63]], writes=[small_tr[63]])
    P.op("pool", lambda E: E.memset(gh[:], 0.0), reads=list(ktrs.values()), writes=[kconst_tr, gh_tr])
    for c in range(8):
        P.op("pool", lambda E, c=c: E.memset(ubuf[:, c, :], 0.0), writes=[ub_tr[c]])
    P.op("dve", lambda E: E.tensor_scalar(out=gh[:, 0:16], in0=cst[:, G_F1POST:G_F1POST + 16], scalar1=0.5,
                                          scalar2=None, op0=ALU.mult), reads=[cst_tr], writes=[gh_tr])
    P.op("dve", lambda E: E.tensor_scalar(out=gh[:, 16:32], in0=cst[:, G_F2POST:G_F2POST + 16], scalar1=0.5,
                                          scalar2=None, op0=ALU.mult), reads=[cst_tr, gh_tr], writes=[gh_tr])

    for pc in range(NPRE):
        load_chunk(pc)
        ffn(W1a, W2a, G_F1PRE, 0)
        rms_rstd(xres_group, 4, D)
        norm_apply(G_MIXPRE)
        make_tables(pc)
        kv_proj(pc)
        if pc == NPRE - 1:
            conv_ab(32, C - 32, 0)
            for c in range(8):
                P.op("dve", lambda E, c=c: E.tensor_scalar(out=ubuf[:, c, 0:32], in0=ubuf[:, c, 0:32],
                                                          scalar1=cst[:, C_VFLAG:C_VFLAG + 1], scalar2=None,
                                                          op0=ALU.mult),
                     reads=[ub_tr[c], cst_tr], writes=[ub_tr[c]])

    for c in range(NOWN):
        cc = NPRE + c
        load_chunk(cc)
        ffn(W1a, W2a, G_F1PRE, 0)
        rms_rstd(xres_group, 4, D)
        norm_apply(G_MIXPRE)
        make_tables(cc)
        Wk, Wk_tr = kv_proj(cc)
        wi_proj(c, Wk, Wk_tr)
        for grp in range(4):
            Wt, Wt_tr = proj_group(2048 + grp * 256, 256)
            for j in range(2):
                ps, ps_tr = proj_ps(Wt, Wt_tr, j)
                rope(ps, ps_tr, 0, qT[2 * grp + j], [qT_tr[2 * grp + j]])
        for grp in range(4):
            Wt, Wt_tr = proj_group(3584 + grp * 256, 256)
            for j in range(2):
                ps, ps_tr = proj_ps(Wt, Wt_tr, j)
                rope(ps, ps_tr, 1, qiT[2 * grp + j], [qiT_tr[2 * grp + j]])
        conv_ab(C, 0, 32)
        conv_branch()
        attn_chunk(c)
        merge_and_out()
        ffn(W1b, W2b, G_F2PRE, 16)
        store_chunk(c)

    sem_names = list(Prog.ENG) + sorted(P.dtot.keys())
    SEM = {n: es.enter_context(nc.semaphore("s_" + n)) for n in sem_names}
    block = es.enter_context(nc.Block())

    def replay(E, name):
        for waits, fn, sem, inc in P.ops[name]:
            for s, v in waits:
                E.wait_ge(SEM[s], v)
            r = fn(E)
            if isinstance(r, (list, tuple)):
                for ins in r:
                    ins.then_inc(SEM[sem], inc)
            else:
                r.then_inc(SEM[sem], inc)

    @block.tensor
    def _(E):
        replay(E, "pe")

    @block.scalar
    def _(E):
        replay(E, "act")

    @block.vector
    def _(E):
        replay(E, "dve")

    @block.gpsimd
    def _(E):
        replay(E, "pool")

    @block.sync
    def _(E):
        replay(E, "sp")
        for n in ("o0", "o1"):
            E.wait_ge(SEM[n], P.dtot[n])

    es.close()
    return nc


_NC_CACHE = {}


def _consts_for_core(half, inputs):
    cst = np.zeros((128, NCST), np.float32)

    def fm(v, ncol):
        return np.ascontiguousarray(np.asarray(v, np.float32).reshape(ncol, 128).T)
    cst[:, G_F1PRE:G_F1PRE + 16] = fm(inputs["ffn1_norm_pre"][0], 16)
    cst[:, G_F1POST:G_F1POST + 16] = fm(inputs["ffn1_norm_post"][0], 16)
    cst[:, G_MIXPRE:G_MIXPRE + 16] = fm(inputs["mix_norm_pre"][0], 16)
    cst[:, G_MIXPOST:G_MIXPOST + 16] = fm(inputs["mix_norm_post"][0], 16)
    cst[:, G_F2PRE:G_F2PRE + 16] = fm(inputs["ffn2_norm_pre"][0], 16)
    cst[:, G_F2POST:G_F2POST + 16] = fm(inputs["ffn2_norm_post"][0], 16)
    dw = np.asarray(inputs["conv_dw"][0], np.float32)
    cst[:, C_DW:C_DW + 248] = dw.reshape(31, 8, 128).transpose(2, 1, 0).reshape(128, 248)
    cst[:, C_DWB:C_DWB + 8] = fm(inputs["conv_dw_b"][0], 8)
    cst[:, C_LNG:C_LNG + 8] = fm(inputs["conv_ln_g"][0], 8)
    cst[:, C_LNB:C_LNB + 8] = fm(inputs["conv_ln_b"][0], 8)
    p = np.arange(128)
    invq = np.where(p < 32, np.power(np.float32(500000.0), -(p % 16).astype(np.float32) * np.float32(2.0 / 32)), 0.0)
    pi_ = p % 64
    invi = np.where(pi_ < 16, np.power(np.float32(500000.0), -(pi_ % 8).astype(np.float32) * np.float32(2.0 / 16)), 0.0)
    cst[:, C_INVF] = (invq / TWO_PI).astype(np.float32)
    cst[:, C_INVF + 1] = (invi / TWO_PI).astype(np.float32)
    cst[:, C_VFLAG] = float(half)
    cst[:, C_NEGBIG] = 0.0 if half else -1e30
    cst[:, C_BIS:C_BIS + NIT + 1] = -(0.5 ** np.arange(NIT + 1, dtype=np.float64)).astype(np.float32)[None, :]
    return cst


def _rmat():
    R = np.zeros((128, 256), np.float32)
    for m in range(16):
        R[m + 16, m] = -1.0
        R[m, m + 16] = 1.0
    for base in (0, 64):
        for m in range(8):
            R[base + m + 8, 128 + base + m] = -1.0
            R[base + m, 128 + base + m + 8] = 1.0
    return R


def kernel(**inputs):
    x = np.asarray(inputs["x"], np.float32)
    pos = np.asarray(inputs["positions"], np.int32)
    if "nc" not in _NC_CACHE:
        _NC_CACHE["nc"] = build_nc()
    nc = _NC_CACHE["nc"]
    wnames = ["ffn1_w1", "ffn1_w2", "w_in", "conv_w_pw", "attn_w_o", "w_out", "ffn2_w1", "ffn2_w2"]
    wts = {n: np.ascontiguousarray(np.asarray(inputs[n], np.float32)[0]) for n in wnames}
    rm = _rmat()
    in_maps = []
    for core in range(8):
        b, half = core // 2, core % 2
        own = slice(half * 2048, (half + 1) * 2048)
        xin = np.ascontiguousarray(np.concatenate([x[b, 0:2048], x[b, own]], axis=0))
        pp = np.concatenate([pos[b, 0:2048], pos[b, own]], axis=0)
        posb = np.ascontiguousarray(np.broadcast_to(pp[None, :], (128, 4096))).astype(np.int32)
        m = {"xin": xin, "posb": posb, "cst": _consts_for_core(half, inputs), "rmat": rm}
        m.update(wts)
        in_maps.append(m)
    res = run_bass_kernel_spmd(nc, in_maps, core_ids=list(range(8)))
    out = np.zeros((4, 4096, 2048), np.float32)
    for core in range(8):
        b, half = core // 2, core % 2
        out[b, half * 2048:(half + 1) * 2048] = np.asarray(res.results[core]["out"], np.float32)
    return out
```

```python
import numpy as np
from contextlib import ExitStack
import concourse.bass as bass
import concourse.mybir as mybir
from concourse.bass_utils import run_bass_kernel_spmd
from concourse.alu_op_type import AluOpType as ALU

F32 = mybir.dt.float32
BF16 = mybir.dt.bfloat16
I32 = mybir.dt.int32
AF = mybir.ActivationFunctionType
AX = mybir.AxisListType

D = 2048
DFF = 5632
NFF = DFF // 128
C = 512
NPRE = 4
NOWN = 4
INW = 8784
EPS = 1e-6
NIT = 16
NR = 3
MAGIC = 12582912.0
ATT_SCALE = 128 ** -0.5
TWO_PI = 6.283185307179586

G_F1PRE, G_F1POST, G_MIXPRE, G_MIXPOST, G_F2PRE, G_F2POST = 0, 16, 32, 48, 64, 80
C_DW = 96
C_DWB = C_DW + 248
C_LNG = C_DWB + 8
C_LNB = C_LNG + 8
C_INVF = C_LNB + 8
C_VFLAG = C_INVF + 2
C_NEGBIG = C_VFLAG + 1
C_BIS = C_NEGBIG + 1
NCST = C_BIS + NIT + 1


class Tr:
    __slots__ = ("w", "r")

    def __init__(self):
        self.w = None
        self.r = {}


class Prog:
    ENG = ("pe", "act", "dve", "pool", "sp")

    def __init__(self):
        self.ops = {e: [] for e in self.ENG}
        self.cnt = {e: 0 for e in self.ENG}
        self.seen = {e: {} for e in self.ENG}
        self.dtot = {}

    def _need(self, eng, reads, writes):
        need = {}

        def add(s, v):
            if need.get(s, 0) < v:
                need[s] = v

        own = 0
        for t in reads:
            if t.w is not None:
                add(*t.w)
                if t.w[0] == eng:
                    own = max(own, t.w[1])
        for t in writes:
            if t.w is not None:
                add(*t.w)
            for s, v in t.r.items():
                add(s, v)
        out = []
        seen = self.seen[eng]
        for s, v in need.items():
            if s == eng:
                continue
            if seen.get(s, 0) < v:
                out.append((s, v))
                seen[s] = v
        if eng != "pe" and own > seen.get(eng, 0):
            out.append((eng, own))
            seen[eng] = own
        return out

    def op(self, eng, fn, reads=(), writes=()):
        waits = self._need(eng, reads, writes)
        self.cnt[eng] += 1
        v = self.cnt[eng]
        for t in reads:
            if t.r.get(eng, 0) < v:
                t.r[eng] = v
        for t in writes:
            t.w = (eng, v)
            t.r = {}
        self.ops[eng].append((waits, fn, eng, 1))

    def dma(self, eng, dsem, fn, reads=(), writes=(), n=1):
        waits = self._need(eng, reads, writes)
        self.dtot[dsem] = self.dtot.get(dsem, 0) + 16 * n
        v = self.dtot[dsem]
        for t in reads:
            if t.r.get(dsem, 0) < v:
                t.r[dsem] = v
        for t in writes:
            t.w = (dsem, v)
            t.r = {}
        self.ops[eng].append((waits, fn, dsem, 16))


def build_nc():
    nc = bass.Bass("TRN2", target_bir_lowering=False)
    P = Prog()

    def din(name, shape, dt=F32):
        return nc.dram_tensor(name, list(shape), dt, kind="ExternalInput").ap()

    xin = din("xin", [(NPRE + NOWN) * C, D])
    posb = din("posb", [128, (NPRE + NOWN) * C], I32)
    cstd = din("cst", [128, NCST])
    rmatd = din("rmat", [128, 256])
    W1a = din("ffn1_w1", [D, 2 * DFF])
    W2a = din("ffn1_w2", [DFF, D])
    Win = din("w_in", [D, INW])
    Wpw = din("conv_w_pw", [1024, D])
    Wo = din("attn_w_o", [1024, D])
    Wout = din("w_out", [D, D])
    W1b = din("ffn2_w1", [D, 2 * DFF])
    W2b = din("ffn2_w2", [DFF, D])
    outd = nc.dram_tensor("out", [NOWN * C, D], F32, kind="ExternalOutput").ap()

    es = ExitStack()

    def sb(name, shape, dt):
        return es.enter_context(nc.sbuf_tensor(name, list(shape), dt))

    ringall = sb("ringall", [128, NR, 4096], BF16)
    ring = [ringall[:, i, :] for i in range(NR)]
    ring_tr = [Tr() for _ in range(NR)]
    xres = sb("xres", [128, 16, C], F32)
    xres_tr = [Tr() for _ in range(16)]
    hT = sb("hT", [128, 16, C], BF16)
    hT_tr = [Tr() for _ in range(16)]
    aT = sb("aT", [128, NFF, C], BF16)
    aT_tr = [Tr() for _ in range(NFF)]
    mixs = sb("mixs", [128, 12, C], F32)
    mix_tr = [Tr() for _ in range(12)]
    Kc = sb("Kc", [128, 2, 4096], BF16)
    Kc_tr = Tr()
    Vc = sb("Vc", [128, 32, 256], BF16)
    Vc_tr = Tr()
    Kic = sb("Kic", [128, 4096], BF16)
    Kic_tr = Tr()
    ubuf = sb("ubuf", [128, 8, 32 + C], BF16)
    ub_tr = [Tr() for _ in range(8)]
    cst = sb("cst_sb", [128, NCST], F32)
    cst_tr = Tr()
    gh = sb("gh", [128, 32], F32)
    gh_tr = Tr()
    rmat = sb("rmat_sb", [128, 256], F32)
    rmat_tr = Tr()
    ones_bf = sb("ones_bf", [128, 128], BF16)
    ident_f = sb("ident_f", [128, 128], F32)
    ident_bf = sb("ident_bf", [128, 128], BF16)
    tri = sb("tri", [128, 128], F32)
    kconst_tr = Tr()
    sgt = [sb(f"sgt{i}", [128, C], F32) for i in range(2)]
    sgt_tr = [Tr() for _ in range(2)]
    rstd_bc = sb("rstd_bc", [128, C], F32)
    rstd_tr = Tr()
    rs_tmp = sb("rs_tmp", [128, C], F32)
    rs_tr = Tr()
    ybase = sb("ybase", [128, C], F32)
    ybase_tr = Tr()
    posi = ybase[:].bitcast(I32)
    posi_tr = ybase_tr
    wabs = sb("wabs", [128, 16, 16], F32)
    wsgn = sb("wsgn", [128, 16, 16], F32)
    wab_tr = [Tr() for _ in range(16)]
    small = sb("small", [128, 64], F32)
    small_tr = [Tr() for _ in range(64)]
    nwt = sb("nwt", [128, 2, NIT + 1], F32)
    nwt_tr = [Tr() for _ in range(2)]
    tot = sb("tot", [128, 2, 128], F32)
    tot_tr = [Tr() for _ in range(2)]

    rl = [sgt[0], sgt[1], rs_tmp, ybase]
    rl_tr = [sgt_tr[0], sgt_tr[1], rs_tr, ybase_tr]
    NPS = 6
    pall = es.enter_context(nc.psum_tensor("pall", [128, 8, C], F32))
    pbank = [pall[:, i, :] for i in range(8)]
    pbank_tr = [Tr() for _ in range(8)]
    pbt = pall[:, 7, :].bitcast(BF16)
    pbt_tr = pbank_tr[7]
    st = {"ps": 0, "ring": 0, "sg": 0, "sg4": 0, "ps2": 0, "ix": 0, "ps3": 0}

    pinned = set()

    def psum(pin=False):
        while True:
            i = st["ps"] % NPS
            st["ps"] += 1
            if i not in pinned:
                break
        if pin:
            pinned.add(i)
        return pbank[i], pbank_tr[i]

    def psum2():
        i0 = 2 * (st["ps2"] % 2)
        st["ps2"] += 1
        return i0

    def unpin(ps):
        for i in range(8):
            if pbank[i] is ps:
                pinned.discard(i)

    def yT_slice(m):
        if m < 12:
            return mixs[:, m, :], [mix_tr[m]]
        i = m - 12
        ap = hT[:, 2 * i:2 * i + 2, :].bitcast(F32).rearrange("p a b -> p (a b)")
        return ap, [hT_tr[2 * i], hT_tr[2 * i + 1]]

    def yT_group(g):
        if g < 3:
            return mixs[:, 4 * g:4 * g + 4, :].rearrange("p a b -> p (a b)"), mix_tr[4 * g:4 * g + 4]
        return hT[:, 0:8, :].bitcast(F32).rearrange("p a b -> p (a b)"), hT_tr[0:8]

    def xres_group(g):
        return xres[:, 4 * g:4 * g + 4, :].rearrange("p a b -> p (a b)"), xres_tr[4 * g:4 * g + 4]

    sq_ap = aT[:, 40:44, :].rearrange("p a b -> p (a b)")
    sq_trs = aT_tr[40:44]
    scores = mixs[:, 0:8, :].rearrange("p a b -> p (a b)")
    scores_tr = mix_tr[0:8]
    scores1 = ringall[:, 1:3, :].bitcast(F32).rearrange("p a b -> p (a b)")
    maskb = mixs[:, 8:12, :].bitcast(BF16).rearrange("p a b -> p (a b)")
    mask_tr = mix_tr[8:12]
    convo = [mixs[:, c, :] for c in range(8)]
    tabs = [mixs[:, 8 + i, :] for i in range(4)]
    tab_tr = mix_tr[8:12]
    maskT = aT[:, 0:8, :].rearrange("p a b -> p (a b)")
    maskT_tr = aT_tr[0:8]
    qiT = [aT[:, 8 + j, :] for j in range(8)]
    qiT_tr = aT_tr[8:16]
    qT = [aT[:, 16 + j, :] for j in range(8)]
    qT_tr = aT_tr[16:24]
    cT = [aT[:, 24 + j, :] for j in range(8)]
    cT_tr = aT_tr[24:32]
    oT = [aT[:, 32 + j, :] for j in range(8)]
    oT_tr = aT_tr[32:40]
    mergedT = [aT[:, j, :] for j in range(16)]
    merged_tr = aT_tr[0:16]

    def xtok(slot):
        ap = aT[:, 8 * slot:8 * slot + 8, :].bitcast(F32).rearrange("p a b -> p (a b)")
        return ap, aT_tr[8 * slot:8 * slot + 8]

    def wtile(W, r0, nk, c0, ncols):
        s = st["ring"] % NR
        st["ring"] += 1
        view = ring[s][:, 0:nk * ncols].rearrange("p (k n) -> p k n", n=ncols)
        src = W[r0 * 128:(r0 + nk) * 128, c0:c0 + ncols].rearrange("(k p) n -> p k n", p=128)
        P.dma("pool", f"w{s}", lambda E, v=view, s_=src: [E.dma_start(out=v, in_=s_)], writes=[ring_tr[s]])
        return view, ring_tr[s]

    def wtile_kiwi():
        s = st["ring"] % NR
        st["ring"] += 1
        view = ring[s][:, 0:16 * 144].rearrange("p (k n) -> p k n", n=144)

        def src(c0, n):
            return Win[:, c0:c0 + n].rearrange("(k p) n -> p k n", p=128)

        def fn(E, v=view):
            return [E.dma_start(out=v[:, :, 0:64], in_=src(4608, 64)),
                    E.dma_start(out=v[:, :, 64:128], in_=src(4608, 64)),
                    E.dma_start(out=v[:, :, 128:144], in_=src(4672, 16))]
        P.dma("pool", f"w{s}", fn, writes=[ring_tr[s]], n=3)
        return view, ring_tr[s]

    def mm_group(ps, ps_tr, pairs, reads, n=C, first=True, last=True):
        def fn(E, ps=ps, pairs=pairs, n=n, first=first, last=last):
            r = None
            for i, (l, rh) in enumerate(pairs):
                r = E.matmul(ps[:, 0:n], lhsT=l, rhs=rh, start=(first and i == 0),
                             stop=(last and i == len(pairs) - 1))
            return r
        P.op("pe", fn, reads=reads, writes=[ps_tr])

    def evac(i, out_ap, in_ap, reads, writes):
        if i % 2 == 0:
            P.op("act", lambda E, o=out_ap, a=in_ap: E.copy(out=o, in_=a), reads=reads, writes=writes)
        else:
            P.op("dve", lambda E, o=out_ap, a=in_ap: E.tensor_copy(out=o, in_=a), reads=reads, writes=writes)

    def rms_rstd(group_fn, ngroups, dim):
        ps, ps_tr = psum()
        for g in range(ngroups):
            src, trs = group_fn(g)
            P.op("act", lambda E, s=src: E.activation(out=sq_ap, in_=s, func=AF.Square),
                 reads=trs, writes=sq_trs)
            pairs = [(ones_bf[:], aT[:, 40 + kk, :]) for kk in range(4)]
            mm_group(ps, ps_tr, pairs, reads=list(sq_trs) + [kconst_tr], first=(g == 0), last=(g == ngroups - 1))
        P.op("act", lambda E, ps=ps: E.activation(out=rs_tmp[:], in_=ps[:], func=AF.Sqrt, scale=1.0 / dim,
                                                   bias=small[:, 63:64]),
             reads=[ps_tr, small_tr[63]], writes=[rs_tr])
        P.op("dve", lambda E: E.reciprocal(out=rstd_bc[:], in_=rs_tmp[:]), reads=[rs_tr], writes=[rstd_tr])

    def norm_apply(gcol):
        for k in range(16):
            P.op("dve", lambda E, k=k: E.scalar_tensor_tensor(
                out=hT[:, k, :], in0=xres[:, k, :], scalar=cst[:, gcol + k:gcol + k + 1],
                in1=rstd_bc[:], op0=ALU.mult, op1=ALU.mult),
                reads=[xres_tr[k], rstd_tr, cst_tr], writes=[hT_tr[k]])

    def residual_update(gtile, gcol):
        for k in range(16):
            yap, ytrs = yT_slice(k)
            i = st["sg"] % 2
            st["sg"] += 1
            P.op("dve", lambda E, k=k, yap=yap, i=i: E.scalar_tensor_tensor(
                out=sgt[i][:], in0=yap, scalar=gtile[:, gcol + k:gcol + k + 1], in1=rstd_bc[:],
                op0=ALU.mult, op1=ALU.mult),
                reads=list(ytrs) + [rstd_tr, cst_tr, gh_tr], writes=[sgt_tr[i]])
            P.op("dve", lambda E, k=k, i=i: E.tensor_tensor(out=xres[:, k, :], in0=xres[:, k, :], in1=sgt[i][:],
                                                             op=ALU.add),
                 reads=[sgt_tr[i], xres_tr[k]], writes=[xres_tr[k]])

    def ffn(W1, W2, gpre, gpost_half_col):
        rms_rstd(xres_group, 4, D)
        norm_apply(gpre)
        hreads = list(hT_tr)
        for f2 in range(NFF // 2):
            Wg, Wg_tr = wtile(W1, 0, 16, f2 * 256, 256)
            Wu, Wu_tr = wtile(W1, 0, 16, DFF + f2 * 256, 256)
            pgs = []
            for j in range(2):
                pg, pg_tr = psum()
                mm_group(pg, pg_tr, [(Wg[:, k, j * 128:(j + 1) * 128], hT[:, k, :]) for k in range(16)],
                         reads=hreads + [Wg_tr])
                pgs.append((pg, pg_tr))
            for j in range(2):
                f = 2 * f2 + j
                pg, pg_tr = pgs[j]
                pu, pu_tr = psum()
                mm_group(pu, pu_tr, [(Wu[:, k, j * 128:(j + 1) * 128], hT[:, k, :]) for k in range(16)],
                         reads=hreads + [Wu_tr])
                i = st["sg"] % 2
                st["sg"] += 1
                P.op("act", lambda E, pg=pg, i=i: E.activation(out=sgt[i][:], in_=pg[:], func=AF.Silu),
                     reads=[pg_tr], writes=[sgt_tr[i]])
                P.op("dve", lambda E, pu=pu, i=i, f=f: E.tensor_tensor(out=aT[:, f, :], in0=sgt[i][:], in1=pu[:],
                                                                       op=ALU.mult),
                     reads=[sgt_tr[i], pu_tr], writes=[aT_tr[f]])
        for m2 in range(8):
            pa, pa_tr = psum()
            pb_, pb_tr = psum()
            for kq in range(4):
                Wt, Wt_tr = wtile(W2, kq * 11, 11, m2 * 256, 256)
                rd = aT_tr[kq * 11:(kq + 1) * 11] + [Wt_tr]
                mm_group(pa, pa_tr, [(Wt[:, kk, 0:128], aT[:, kq * 11 + kk, :]) for kk in range(11)], reads=rd,
                         first=(kq == 0), last=(kq == 3))
                mm_group(pb_, pb_tr, [(Wt[:, kk, 128:256], aT[:, kq * 11 + kk, :]) for kk in range(11)], reads=rd,
                         first=(kq == 0), last=(kq == 3))
            ya, ya_trs = yT_slice(2 * m2)
            yb, yb_trs = yT_slice(2 * m2 + 1)
            evac(0, ya, pa[:], [pa_tr], ya_trs)
            evac(1, yb, pb_[:], [pb_tr], yb_trs)
        rms_rstd(yT_group, 4, D)
        residual_update(gh, gpost_half_col)

    def load_chunk(cc):
        for tt in range(4):
            slot = (cc * 4 + tt) % 2
            xt, xt_trs = xtok(slot)
            row0 = (cc * 4 + tt) * 128
            P.dma("sp", f"x{slot}", lambda E, xt=xt, row0=row0: [E.dma_start(out=xt, in_=xin[row0:row0 + 128, :])],
                  writes=xt_trs)
            for kg in range(4):
                ps, ps_tr = psum()

                def fn(E, ps=ps, xt=xt, kg=kg):
                    r = None
                    for kk in range(4):
                        r = E.transpose(out=ps[:, kk * 128:(kk + 1) * 128],
                                        in_=xt[:, (kg * 4 + kk) * 128:(kg * 4 + kk + 1) * 128], identity=ident_f[:])
                    return r
                P.op("pe", fn, reads=list(xt_trs) + [kconst_tr], writes=[ps_tr])
                evac(kg, xres[:, kg * 4:kg * 4 + 4, tt * 128:(tt + 1) * 128],
                     ps[:].rearrange("p (a b) -> p a b", a=4), [ps_tr], xres_tr[kg * 4:kg * 4 + 4])

    def store_chunk(c):
        for tt in range(4):
            slot = tt % 2
            xt, xt_trs = xtok(slot)
            for kg in range(4):
                ps, ps_tr = psum()

                def fn(E, ps=ps, kg=kg, tt=tt):
                    r = None
                    for kk in range(4):
                        r = E.transpose(out=ps[:, kk * 128:(kk + 1) * 128],
                                        in_=xres[:, kg * 4 + kk, tt * 128:(tt + 1) * 128], identity=ident_f[:])
                    return r
                P.op("pe", fn, reads=xres_tr[kg * 4:kg * 4 + 4] + [kconst_tr], writes=[ps_tr])
                evac(kg, xt[:, kg * 512:(kg + 1) * 512], ps[:], [ps_tr], xt_trs[2 * kg:2 * kg + 2])
            row0 = (c * 4 + tt) * 128
            P.dma("sp", f"o{slot}", lambda E, xt=xt, row0=row0: [E.dma_start(out=outd[row0:row0 + 128, :], in_=xt)],
                  reads=xt_trs)

    def make_tables(cc):
        P.dma("sp", "pos", lambda E, cc=cc: [E.dma_start(out=posi, in_=posb[:, cc * C:(cc + 1) * C])],
              writes=[posi_tr])
        P.op("dve", lambda E: E.tensor_copy(out=rs_tmp[:], in_=posi), reads=[posi_tr], writes=[rs_tr])
        for typ in range(2):
            P.op("dve", lambda E, typ=typ: E.tensor_scalar(out=ybase[:], in0=rs_tmp[:],
                                                          scalar1=cst[:, C_INVF + typ:C_INVF + typ + 1],
                                                          scalar2=None, op0=ALU.mult),
                 reads=[rs_tr, cst_tr], writes=[ybase_tr])
            for cs in range(2):
                dst = tabs[2 * typ + cs]
                dtr = [tab_tr[2 * typ + cs]]
                i = st["sg"] % 2
                st["sg"] += 1
                shift = 0.25 if cs == 0 else 0.0
                P.op("dve", lambda E, i=i, shift=shift: E.tensor_scalar(out=sgt[i][:], in0=ybase[:], scalar1=shift,
                                                                       scalar2=None, op0=ALU.add),
                     reads=[ybase_tr], writes=[sgt_tr[i]])
                P.op("dve", lambda E, i=i, dst=dst: E.tensor_scalar(out=dst, in0=sgt[i][:], scalar1=MAGIC,
                                                                   scalar2=MAGIC, op0=ALU.add, op1=ALU.subtract),
                     reads=[sgt_tr[i]], writes=dtr)
                P.op("dve", lambda E, i=i, dst=dst: E.tensor_tensor(out=dst, in0=sgt[i][:], in1=dst, op=ALU.subtract),
                     reads=[sgt_tr[i]] + dtr, writes=dtr)
                P.op("dve", lambda E, dst=dst: E.tensor_scalar(out=dst, in0=dst, scalar1=0.4999995,
                                                              scalar2=-0.4999995, op0=ALU.min, op1=ALU.max),
                     reads=dtr, writes=dtr)
                P.op("act", lambda E, dst=dst: E.activation(out=dst, in_=dst, func=AF.Sin, scale=TWO_PI),
                     reads=dtr, writes=dtr)

    def rope(ps, ps_tr, typ, dst_ap, dst_trs):
        i = st["sg"] % 2
        st["sg"] += 1
        P.op("act", lambda E, ps=ps, i=i: E.copy(out=sgt[i][:], in_=ps[:]), reads=[ps_tr], writes=[sgt_tr[i]])
        pr, pr_tr = psum()
        mm_group(pr, pr_tr, [(rmat[:, typ * 128:(typ + 1) * 128], sgt[i][:])], reads=[sgt_tr[i], rmat_tr])
        P.op("dve", lambda E, pr=pr, typ=typ: E.tensor_tensor(out=rs_tmp[:], in0=pr[:], in1=tabs[2 * typ + 1],
                                                             op=ALU.mult),
             reads=[pr_tr, tab_tr[2 * typ + 1]], writes=[rs_tr])
        P.op("dve", lambda E, i=i, typ=typ: E.tensor_tensor(out=sgt[i][:], in0=sgt[i][:], in1=tabs[2 * typ],
                                                           op=ALU.mult),
             reads=[sgt_tr[i], tab_tr[2 * typ]], writes=[sgt_tr[i]])
        P.op("dve", lambda E, i=i, dst_ap=dst_ap: E.tensor_tensor(out=dst_ap, in0=sgt[i][:], in1=rs_tmp[:],
                                                                 op=ALU.add),
             reads=[sgt_tr[i], rs_tr], writes=dst_trs)

    def proj_group(c0, ncols):
        Wt, Wt_tr = wtile(Win, 0, 16, c0, ncols)
        return Wt, Wt_tr

    def proj_ps(Wt, Wt_tr, j, n=C, tok0=0):
        ps, ps_tr = psum()
        mm_group(ps, ps_tr, [(Wt[:, k, j * 128:(j + 1) * 128], hT[:, k, tok0:tok0 + n]) for k in range(16)],
                 reads=list(hT_tr) + [Wt_tr], n=n)
        return ps, ps_tr

    def kv_proj(cc):
        k0 = cc * C
        Wt, Wt_tr = proj_group(3072, 256)
        for g in range(2):
            ps, ps_tr = proj_ps(Wt, Wt_tr, g)
            rope(ps, ps_tr, 0, Kc[:, g, k0:k0 + C], [Kc_tr])
        Wt, Wt_tr = proj_group(3328, 256)
        for tt in range(4):
            ps, ps_tr = psum()
            mm_group(ps, ps_tr, [(hT[:, k, tt * 128:(tt + 1) * 128], Wt[:, k, :]) for k in range(16)],
                     reads=list(hT_tr) + [Wt_tr], n=256)
            evac(tt, Vc[:, cc * 4 + tt, :], ps[:, 0:256], [ps_tr], [Vc_tr])
        Wk, Wk_tr = wtile_kiwi()
        ps, ps_tr = proj_ps(Wk, Wk_tr, 0)
        rope(ps, ps_tr, 1, Kic[:, k0:k0 + C], [Kic_tr])
        return Wk, Wk_tr

    def wi_proj(c, Wk, Wk_tr):
        for r in range(4):
            i = 4 * c + r
            ps, ps_tr = psum()
            mm_group(ps, ps_tr, [(hT[:, k, r * 128:(r + 1) * 128], Wk[:, k, 128:144]) for k in range(16)],
                     reads=list(hT_tr) + [Wk_tr], n=16)
            P.op("act", lambda E, ps=ps, i=i: E.copy(out=wabs[:, i, :], in_=ps[:, 0:16]),
                 reads=[ps_tr], writes=[wab_tr[i]])

    def conv_ab(n, tok0, dst_off):
        for grp in range(4):
            Wt, Wt_tr = proj_group(1024 + grp * 256, 256)
            for j in range(2):
                c = 2 * grp + j
                ps, ps_tr = proj_ps(Wt, Wt_tr, j, n=n, tok0=tok0)
                P.op("act", lambda E, ps=ps, c=c: E.activation(out=ubuf[:, c, dst_off:dst_off + n], in_=ps[:, 0:n],
                                                               func=AF.Sigmoid),
                     reads=[ps_tr], writes=[ub_tr[c]])
        for grp in range(4):
            Wt, Wt_tr = proj_group(grp * 256, 256)
            for j in range(2):
                c = 2 * grp + j
                ps, ps_tr = proj_ps(Wt, Wt_tr, j, n=n, tok0=tok0)
                P.op("dve", lambda E, ps=ps, c=c: E.tensor_tensor(out=ubuf[:, c, dst_off:dst_off + n],
                                                                  in0=ubuf[:, c, dst_off:dst_off + n],
                                                                  in1=ps[:, 0:n], op=ALU.mult),
                     reads=[ps_tr, ub_tr[c]], writes=[ub_tr[c]])

    diag = aT[:, 40:44, :].rearrange("p a b -> p (a b)").rearrange("p (t n) -> p t n", n=128)

    def conv_branch():
        for c in range(8):
            pc_, pc_tr = psum(pin=True)
            for g in range(8):
                taps = list(range(4 * g, min(4 * g + 4, 31)))
                dtr = aT_tr[40 + g % 4]
                for jj, j in enumerate(taps):
                    P.op("dve", lambda E, c=c, j=j, t=(g % 4) * 4 + jj: E.tensor_scalar(
                        out=diag[:, t, :], in0=ident_bf[:], scalar1=cst[:, C_DW + c * 31 + j:C_DW + c * 31 + j + 1],
                        scalar2=None, op0=ALU.mult),
                        reads=[kconst_tr, cst_tr], writes=[dtr])
                mm_group(pc_, pc_tr, [(diag[:, (g % 4) * 4 + jj, :], ubuf[:, c, 2 + j:2 + j + C])
                                      for jj, j in enumerate(taps)],
                         reads=[dtr, ub_tr[c]], first=(g == 0), last=(g == 7))
            unpin(pc_)
            P.op("act", lambda E, pc_=pc_, c=c: E.activation(out=cT[c], in_=pc_[:], func=AF.Identity,
                                                              bias=cst[:, C_DWB + c:C_DWB + c + 1]),
                 reads=[pc_tr, cst_tr], writes=[cT_tr[c]])
        for c in range(8):
            P.op("dve", lambda E, c=c: E.tensor_copy(out=ubuf[:, c, 0:32], in_=ubuf[:, c, C:C + 32]),
                 reads=[ub_tr[c]], writes=[ub_tr[c]])
        pm, pm_tr = psum(pin=True)
        pq, pq_tr = psum(pin=True)
        for g in range(2):
            src = aT[:, 24 + 4 * g:24 + 4 * g + 4, :].rearrange("p a b -> p (a b)")
            mm_group(pm, pm_tr, [(ones_bf[:], cT[4 * g + kk]) for kk in range(4)],
                     reads=list(cT_tr[4 * g:4 * g + 4]) + [kconst_tr], first=(g == 0), last=(g == 1))
            P.op("act", lambda E, src=src: E.activation(out=sq_ap, in_=src, func=AF.Square),
                 reads=cT_tr[4 * g:4 * g + 4], writes=sq_trs)
            mm_group(pq, pq_tr, [(ones_bf[:], aT[:, 40 + kk, :]) for kk in range(4)],
                     reads=list(sq_trs) + [kconst_tr], first=(g == 0), last=(g == 1))
        unpin(pm)
        unpin(pq)
        P.op("act", lambda E, pm=pm: E.activation(out=ybase[:], in_=pm[:], func=AF.Copy, scale=1.0 / 1024),
             reads=[pm_tr], writes=[ybase_tr])
        P.op("dve", lambda E: E.tensor_tensor(out=rs_tmp[:], in0=ybase[:], in1=ybase[:], op=ALU.mult),
             reads=[ybase_tr], writes=[rs_tr])
        P.op("dve", lambda E, pq=pq: E.scalar_tensor_tensor(out=rs_tmp[:], in0=pq[:], scalar=1.0 / 1024,
                                                           in1=rs_tmp[:], op0=ALU.mult, op1=ALU.subtract),
             reads=[pq_tr, rs_tr], writes=[rs_tr])
        P.op("act", lambda E: E.activation(out=rs_tmp[:], in_=rs_tmp[:], func=AF.Sqrt, bias=small[:, 63:64]),
             reads=[rs_tr, small_tr[63]], writes=[rs_tr])
        P.op("dve", lambda E: E.reciprocal(out=rstd_bc[:], in_=rs_tmp[:]), reads=[rs_tr], writes=[rstd_tr])
        for c in range(8):
            i = st["sg"] % 2
            st["sg"] += 1
            P.op("dve", lambda E, c=c, i=i: E.tensor_tensor(out=sgt[i][:], in0=cT[c], in1=ybase[:], op=ALU.subtract),
                 reads=[cT_tr[c], ybase_tr], writes=[sgt_tr[i]])
            P.op("dve", lambda E, i=i: E.tensor_tensor(out=sgt[i][:], in0=sgt[i][:], in1=rstd_bc[:], op=ALU.mult),
                 reads=[sgt_tr[i], rstd_tr], writes=[sgt_tr[i]])
            P.op("act", lambda E, c=c, i=i: E.activation(out=cT[c], in_=sgt[i][:], func=AF.Silu,
                                                         scale=cst[:, C_LNG + c:C_LNG + c + 1],
                                                         bias=cst[:, C_LNB + c:C_LNB + c + 1]),
                 reads=[sgt_tr[i], cst_tr], writes=[cT_tr[c]])

    class Tile:
        def __init__(self, c, r):
            self.c, self.r = c, r
            self.i = 4 * c + r
            self.qb = NPRE * C + 128 * self.i
            self.NK = self.qb + 128
            self.nblk = (self.NK + 511) // 512
            self.nkt = self.NK // 128
            self.pp = self.i % 2
            if self.pp == 0:
                self.sc = scores
                self.btr = lambda b: scores_tr[b]
            else:
                self.sc = scores1
                self.btr = lambda b: ring_tr[1 + b // 4]
            self.ntr = list({id(self.btr(b)): self.btr(b) for b in range(self.nblk)}.values())

        def sm(self, j):
            return small[:, self.pp * 24 + j:self.pp * 24 + j + 1]

        def smt(self, j):
            return small_tr[self.pp * 24 + j]

    ixb = [rs_tmp, ybase]
    ixb_tr = [rs_tr, ybase_tr]

    def indexer_gen(T):
        i, r, NK, nblk, qb = T.i, T.r, T.NK, T.nblk, T.qb
        sm, smt, sc = T.sm, T.smt, T.sc
        for h in range(16):
            P.op("dve", lambda E, h=h: E.tensor_scalar(out=diag[:, h, :], in0=ident_bf[:],
                                                      scalar1=wabs[:, i, h:h + 1], scalar2=None, op0=ALU.mult),
                 reads=[kconst_tr, wab_tr[i]], writes=[aT_tr[40 + h // 4]])
        for b in range(nblk):
            wd = min(512, NK - 512 * b)
            pacc, pacc_tr = pbank[6], pbank_tr[6]

            def emit_dots(jp, b=b, wd=wd):
                i0 = psum2()

                def fn(E, i0=i0, jp=jp, b=b, wd=wd):
                    E.matmul(pbank[i0][:, 0:wd], lhsT=qiT[jp][0:64, r * 128:(r + 1) * 128],
                             rhs=Kic[0:64, 512 * b:512 * b + wd], start=True, stop=True)
                    return E.matmul(pbank[i0 + 1][:, 0:wd], lhsT=qiT[jp][64:128, r * 128:(r + 1) * 128],
                                    rhs=Kic[64:128, 512 * b:512 * b + wd], start=True, stop=True)
                P.op("pe", fn, reads=[qiT_tr[jp], Kic_tr], writes=[pbank_tr[i0], pbank_tr[i0 + 1]])
                return i0
            nxt = emit_dots(0)
            for jp in range(8):
                i0 = nxt
                if jp + 1 < 8:
                    nxt = emit_dots(jp + 1)
                k = st["ix"] % 2
                st["ix"] += 1
                rlb = ixb[k][:].bitcast(BF16).rearrange("p (a b) -> p a b", a=2)
                P.op("act", lambda E, i0=i0, rlb=rlb, wd=wd: E.activation(
                    out=rlb[:, :, 0:wd], in_=pall[:, i0:i0 + 2, 0:wd], func=AF.Relu),
                    reads=[pbank_tr[i0], pbank_tr[i0 + 1]], writes=[ixb_tr[k]])

                def fn2(E, jp=jp, rlb=rlb, wd=wd, pacc=pacc):
                    E.matmul(pacc[:, 0:wd], lhsT=diag[:, 2 * jp, :], rhs=rlb[:, 0, 0:wd], start=(jp == 0), stop=False)
                    return E.matmul(pacc[:, 0:wd], lhsT=diag[:, 2 * jp + 1, :], rhs=rlb[:, 1, 0:wd], start=False,
                                    stop=(jp == 7))
                P.op("pe", fn2, reads=[ixb_tr[k], aT_tr[40 + (2 * jp) // 4]], writes=[pacc_tr])
                if jp == 7:
                    P.op("dve", lambda E, b=b, wd=wd, pacc=pacc: E.tensor_copy(out=sc[:, 512 * b:512 * b + wd],
                                                                              in_=pacc[:, 0:wd]),
                         reads=[pacc_tr], writes=[T.btr(b)])
                yield
        ntr = T.ntr
        P.op("dve", lambda E: E.tensor_reduce(out=sm(0), in_=sc[:, 0:NK], axis=AX.X, op=ALU.max,
                                              apply_absolute_value=True),
             reads=ntr, writes=[smt(0)])
        P.op("dve", lambda E: E.tensor_scalar(out=sm(1), in0=sm(0), scalar1=1.001, scalar2=1e-30, op0=ALU.mult,
                                              op1=ALU.add),
             reads=[smt(0)], writes=[smt(1)])
        P.op("dve", lambda E: E.tensor_scalar(out=nwt[:, T.pp, :], in0=cst[:, C_BIS:C_BIS + NIT + 1], scalar1=sm(1),
                                              scalar2=None, op0=ALU.mult),
             reads=[smt(1), cst_tr], writes=[nwt_tr[T.pp]])
        ptr = list({id(T.btr(b)): T.btr(b) for b in range(4)}.values())
        P.op("dve", lambda E: E.tensor_scalar(out=sc[:, 0:NPRE * C], in0=sc[:, 0:NPRE * C],
                                              scalar1=cst[:, C_VFLAG:C_VFLAG + 1],
                                              scalar2=cst[:, C_NEGBIG:C_NEGBIG + 1], op0=ALU.mult, op1=ALU.add),
             reads=ptr + [cst_tr], writes=ptr)
        dtr = [T.btr(qb // 512)]
        P.op("dve", lambda E: E.tensor_tensor(out=sc[:, qb:qb + 128], in0=sc[:, qb:qb + 128], in1=tri[:],
                                              op=ALU.add),
             reads=dtr + [kconst_tr], writes=dtr)
        P.op("dve", lambda E: E.memset(sm(2), 0.0), writes=[smt(2)])
        cthr = -(512.0 - NK) + 0.5
        P.op("dve", lambda E: E.memset(sm(6), cthr), writes=[smt(6)])
        yield

    def bisect_gen(T):
        NK, sc, ntr = T.NK, T.sc, T.ntr
        sm, smt = T.sm, T.smt
        for it in range(NIT):
            cur, nxt = 2 + (it % 2), 2 + ((it + 1) % 2)
            if it % 4 == 0:
                P.op("act", lambda E, cur=cur: E.activation(out=maskb[:, 0:NK], in_=sc[:, 0:NK], func=AF.Sign,
                                                            bias=sm(cur), accum_out=sm(4)),
                     reads=list(ntr) + [smt(cur)], writes=list(mask_tr) + [smt(4)])
                P.op("act", lambda E: E.activation(out=sm(5), in_=sm(4), func=AF.Sign, bias=sm(6)),
                     reads=[smt(4), smt(6)], writes=[smt(5)])
            else:
                P.op("dve", lambda E, cur=cur: E.tensor_scalar(out=sm(8), in0=sm(cur), scalar1=-1.0,
                                                              scalar2=None, op0=ALU.mult),
                     reads=[smt(cur)], writes=[smt(8)])
                P.op("dve", lambda E: E.tensor_scalar(out=maskb[:, 0:NK], in0=sc[:, 0:NK],
                                                      scalar1=sm(8), scalar2=0.0, op0=ALU.is_ge,
                                                      op1=ALU.add, accum_out=sm(4)),
                     reads=list(ntr) + [smt(8)], writes=list(mask_tr) + [smt(4)])
                P.op("dve", lambda E: E.tensor_scalar(out=sm(5), in0=sm(4), scalar1=255.5, scalar2=2.0,
                                                      op0=ALU.is_ge, op1=ALU.mult),
                     reads=[smt(4)], writes=[smt(5)])
                P.op("dve", lambda E: E.tensor_scalar(out=sm(5), in0=sm(5), scalar1=-1.0, scalar2=None,
                                                      op0=ALU.add),
                     reads=[smt(5)], writes=[smt(5)])
            if it % 4 == 0:
                P.op("act", lambda E, cur=cur, nxt=nxt, it=it: E.activation(
                    out=sm(nxt), in_=sm(5), func=AF.Identity, scale=nwt[:, T.pp, it + 1:it + 2], bias=sm(cur)),
                    reads=[smt(5), smt(cur), nwt_tr[T.pp]], writes=[smt(nxt)])
            else:
                P.op("dve", lambda E, cur=cur, nxt=nxt, it=it: E.scalar_tensor_tensor(
                    out=sm(nxt), in0=sm(5), scalar=nwt[:, T.pp, it + 1:it + 2], in1=sm(cur),
                    op0=ALU.mult, op1=ALU.add),
                    reads=[smt(5), smt(cur), nwt_tr[T.pp]], writes=[smt(nxt)])
            yield

    def mask_and_T(T):
        NK, nkt, sc, ntr = T.NK, T.nkt, T.sc, T.ntr
        sm, smt = T.sm, T.smt
        fin = 2 + (NIT % 2)
        P.op("dve", lambda E: E.tensor_scalar(out=sm(7), in0=sm(fin), scalar1=-1.0,
                                              scalar2=nwt[:, T.pp, NIT:NIT + 1], op0=ALU.mult, op1=ALU.add),
             reads=[smt(fin), nwt_tr[T.pp]], writes=[smt(7)])
        P.op("dve", lambda E: E.tensor_scalar(out=maskb[:, 0:NK], in0=sc[:, 0:NK], scalar1=sm(7), scalar2=None,
                                              op0=ALU.is_ge),
             reads=list(ntr) + [smt(7)], writes=mask_tr)
        for j0 in range(0, nkt, 8):
            nj = min(8, nkt - j0)

            def fn(E, j0=j0, nj=nj):
                r_ = None
                for jj in range(nj):
                    r_ = E.transpose(out=pbt[:, jj * 128:(jj + 1) * 128],
                                     in_=maskb[:, (j0 + jj) * 128:(j0 + jj + 1) * 128], identity=ident_bf[:])
                return r_
            P.op("pe", fn, reads=list(mask_tr) + [kconst_tr], writes=[pbt_tr])
            evac(j0 // 8, maskT[:, j0 * 128:(j0 + nj) * 128], pbt[:, 0:nj * 128], [pbt_tr], maskT_tr)

    def attention_gen(T):
        r, nkt = T.r, T.nkt
        ngrp = (nkt + 3) // 4
        steps = [(h, kg) for h in range(8) for kg in range(ngrp)]
        pacc7, pacc7_tr = pbank[7], pbank_tr[7]

        def emit_st(h, kg):
            g = h // 4
            j0 = 4 * kg
            nj = min(4, nkt - j0)
            ib = 4 + (st["ps3"] % 2)
            st["ps3"] += 1
            pS, pS_tr = pbank[ib], pbank_tr[ib]

            def fn(E, pS=pS, j0=j0, nj=nj, h=h, g=g):
                r_ = None
                for jj in range(nj):
                    r_ = E.matmul(pS[:, jj * 128:(jj + 1) * 128],
                                  lhsT=Kc[:, g, (j0 + jj) * 128:(j0 + jj + 1) * 128],
                                  rhs=qT[h][:, r * 128:(r + 1) * 128], start=True, stop=True)
                return r_
            P.op("pe", fn, reads=[Kc_tr, qT_tr[h]], writes=[pS_tr])
            return pS, pS_tr

        nxt = emit_st(*steps[0])
        for si, (h, kg) in enumerate(steps):
            g = h // 4
            pS, pS_tr = nxt
            if si + 1 < len(steps):
                nxt = emit_st(*steps[si + 1])
            j0 = 4 * kg
            nj = min(4, nkt - j0)
            wd = nj * 128
            k = st["sg"] % 2
            st["sg"] += 1
            pt = sgt[k][:].bitcast(BF16)
            P.op("act", lambda E, pS=pS, pt=pt, wd=wd: E.activation(out=pt[:, 0:wd], in_=pS[:, 0:wd], func=AF.Exp,
                                                                    scale=ATT_SCALE),
                 reads=[pS_tr], writes=[sgt_tr[k]])
            P.op("pool", lambda E, pt=pt, wd=wd, j0=j0: E.tensor_tensor(
                out=pt[:, 512:512 + wd], in0=pt[:, 0:wd], in1=maskT[:, j0 * 128:j0 * 128 + wd], op=ALU.mult),
                reads=[sgt_tr[k]] + list(maskT_tr), writes=[sgt_tr[k]])

            def fn2(E, pt=pt, j0=j0, nj=nj, g=g, kg=kg):
                r_ = None
                for jj in range(nj):
                    E.matmul(pacc7[:, 0:128], lhsT=Vc[:, j0 + jj, g * 128:(g + 1) * 128],
                             rhs=pt[:, 512 + jj * 128:512 + (jj + 1) * 128],
                             start=(kg == 0 and jj == 0), stop=False, skip_group_check=True)
                    r_ = E.matmul(pacc7[:, 128:256], lhsT=ones_bf[:], rhs=pt[:, 512 + jj * 128:512 + (jj + 1) * 128],
                                  start=False, stop=(kg == ngrp - 1 and jj == nj - 1), skip_group_check=True)
                return r_
            P.op("pe", fn2, reads=[Vc_tr, sgt_tr[k], kconst_tr], writes=[pacc7_tr])
            if kg == ngrp - 1:
                tk = h % 2
                P.op("dve", lambda E, tk=tk: E.reciprocal(out=tot[:, tk, :], in_=pacc7[:, 128:256]),
                     reads=[pacc7_tr], writes=[tot_tr[tk]])
                P.op("dve", lambda E, tk=tk, h=h: E.tensor_tensor(out=oT[h][:, r * 128:(r + 1) * 128],
                                                                 in0=pacc7[:, 0:128], in1=tot[:, tk, :],
                                                                 op=ALU.mult),
                     reads=[pacc7_tr, tot_tr[tk]], writes=[oT_tr[h]])
            yield

    def run_streams(streams):
        done = [0] * len(streams)
        alive = set(range(len(streams)))
        while alive:
            j = min(alive, key=lambda j: (done[j] + 1) / float(streams[j][1]))
            try:
                next(streams[j][0])
                done[j] += 1
            except StopIteration:
                alive.discard(j)

    def attn_chunk(c):
        tiles = [Tile(c, r) for r in range(4)]
        run_streams([(indexer_gen(tiles[0]), 8 * tiles[0].nblk + 1)])
        for r in range(4):
            T = tiles[r]
            streams = [(bisect_gen(T), NIT)]
            if r + 1 < 4:
                streams.append((indexer_gen(tiles[r + 1]), 8 * tiles[r + 1].nblk + 1))
            if r >= 1:
                streams.append((attention_gen(tiles[r - 1]), 8 * ((tiles[r - 1].nkt + 3) // 4)))
            run_streams(streams)
            mask_and_T(T)
        run_streams([(attention_gen(tiles[3]), 8 * ((tiles[3].nkt + 3) // 4))])

    def merge_and_out():
        for m2 in range(8):
            Wp, Wp_tr = wtile(Wpw, 0, 8, m2 * 256, 256)
            Wa, Wa_tr = wtile(Wo, 0, 8, m2 * 256, 256)
            for j in range(2):
                pyc, pyc_tr = psum()
                pya, pya_tr = psum()
                mm_group(pyc, pyc_tr, [(Wp[:, k, j * 128:(j + 1) * 128], cT[k]) for k in range(8)],
                         reads=list(cT_tr) + [Wp_tr])
                mm_group(pya, pya_tr, [(Wa[:, k, j * 128:(j + 1) * 128], oT[k]) for k in range(8)],
                         reads=list(oT_tr) + [Wa_tr])
                st.setdefault("ycya", []).append((pyc, pyc_tr, pya, pya_tr))
            Wg0, Wg0_tr = wtile(Win, 0, 16, 4688 + m2 * 256, 256)
            Wg1, Wg1_tr = wtile(Win, 0, 16, 4688 + 2048 + m2 * 256, 256)
            for j in range(2):
                m = 2 * m2 + j
                pyc, pyc_tr, pya, pya_tr = st["ycya"].pop(0)
                pg0, pg0_tr = proj_ps(Wg0, Wg0_tr, j)
                k0 = st["sg"] % 2
                st["sg"] += 1
                P.op("act", lambda E, pg0=pg0, k0=k0: E.activation(out=sgt[k0][:], in_=pg0[:], func=AF.Sigmoid),
                     reads=[pg0_tr], writes=[sgt_tr[k0]])
                P.op("dve", lambda E, pyc=pyc, k0=k0: E.tensor_tensor(out=sgt[k0][:], in0=sgt[k0][:], in1=pyc[:],
                                                                     op=ALU.mult),
                     reads=[sgt_tr[k0], pyc_tr], writes=[sgt_tr[k0]])
                pg1, pg1_tr = proj_ps(Wg1, Wg1_tr, j)
                P.op("act", lambda E, pg1=pg1: E.activation(out=rs_tmp[:], in_=pg1[:], func=AF.Sigmoid),
                     reads=[pg1_tr], writes=[rs_tr])
                P.op("dve", lambda E, pya=pya: E.tensor_tensor(out=rs_tmp[:], in0=rs_tmp[:], in1=pya[:], op=ALU.mult),
                     reads=[rs_tr, pya_tr], writes=[rs_tr])
                P.op("dve", lambda E, k0=k0, m=m: E.tensor_tensor(out=mergedT[m], in0=sgt[k0][:], in1=rs_tmp[:],
                                                                 op=ALU.add),
                     reads=[sgt_tr[k0], rs_tr], writes=[merged_tr[m]])
        for m2 in range(8):
            Wt, Wt_tr = wtile(Wout, 0, 16, m2 * 256, 256)
            for j in range(2):
                m = 2 * m2 + j
                ps, ps_tr = psum()
                mm_group(ps, ps_tr, [(Wt[:, k, j * 128:(j + 1) * 128], mergedT[k]) for k in range(16)],
                         reads=list(merged_tr) + [Wt_tr])
                ya, ya_trs = yT_slice(m)
                evac(m, ya, ps[:], [ps_tr], ya_trs)
        rms_rstd(yT_group, 4, D)
        residual_update(cst, G_MIXPOST)

    P.dma("sp", "c0", lambda E: [E.dma_start(out=cst[:], in_=cstd), E.dma_start(out=rmat[:], in_=rmatd)],
          writes=[cst_tr, rmat_tr], n=2)

    ktrs = {n: Tr() for n in ("ones", "identf", "identb", "tri")}
    P.op("pool", lambda E: E.memset(ones_bf[:], 1.0), writes=[ktrs["ones"]])
    P.op("pool", lambda E: E.memset(ident_f[:], 0.0), writes=[ktrs["identf"]])
    P.op("pool", lambda E: E.affine_select(out=ident_f[:], in_=ident_f[:], pattern=[[-1, 128]],
                                           compare_op=ALU.not_equal, fill=1.0, base=0, channel_multiplier=1),
         reads=[ktrs["identf"]], writes=[ktrs["identf"]])
    P.op("pool", lambda E: E.memset(ident_bf[:], 0.0), writes=[ktrs["identb"]])
    P.op("pool", lambda E: E.affine_select(out=ident_bf[:], in_=ident_bf[:], pattern=[[-1, 128]],
                                           compare_op=ALU.not_equal, fill=1.0, base=0, channel_multiplier=1),
         reads=[ktrs["identb"]], writes=[ktrs["identb"]])
    P.op("pool", lambda E: E.memset(tri[:], 0.0), writes=[ktrs["tri"]])
    P.op("pool", lambda E: E.affine_select(out=tri[:], in_=tri[:], pattern=[[-1, 128]], compare_op=ALU.is_ge,
                                           fill=-1e30, base=0, channel_multiplier=1),
         reads=[ktrs["tri"]], writes=[ktrs["tri"]])
    P.op("pool", lambda E: E.memset(small[:], 0.0), writes=small_tr)
    P.op("pool", lambda E: E.memset(small[:, 63:64], EPS), reads=[small_tr[63]], writes=[small_tr[63]])
    P.op("pool", lambda E: E.memset(gh[:], 0.0), reads=list(ktrs.values()), writes=[kconst_tr, gh_tr])
    for c in range(8):
        P.op("pool", lambda E, c=c: E.memset(ubuf[:, c, :], 0.0), writes=[ub_tr[c]])
    P.op("dve", lambda E: E.tensor_scalar(out=gh[:, 0:16], in0=cst[:, G_F1POST:G_F1POST + 16], scalar1=0.5,
                                          scalar2=None, op0=ALU.mult), reads=[cst_tr], writes=[gh_tr])
    P.op("dve", lambda E: E.tensor_scalar(out=gh[:, 16:32], in0=cst[:, G_F2POST:G_F2POST + 16], scalar1=0.5,
                                          scalar2=None, op0=ALU.mult), reads=[cst_tr, gh_tr], writes=[gh_tr])

    for pc in range(NPRE):
        load_chunk(pc)
        ffn(W1a, W2a, G_F1PRE, 0)
        rms_rstd(xres_group, 4, D)
        norm_apply(G_MIXPRE)
        make_tables(pc)
        kv_proj(pc)
        if pc == NPRE - 1:
            conv_ab(32, C - 32, 0)
            for c in range(8):
                P.op("dve", lambda E, c=c: E.tensor_scalar(out=ubuf[:, c, 0:32], in0=ubuf[:, c, 0:32],
                                                          scalar1=cst[:, C_VFLAG:C_VFLAG + 1], scalar2=None,
                                                          op0=ALU.mult),
                     reads=[ub_tr[c], cst_tr], writes=[ub_tr[c]])

    for c in range(NOWN):
        cc = NPRE + c
        load_chunk(cc)
        ffn(W1a, W2a, G_F1PRE, 0)
        rms_rstd(xres_group, 4, D)
        norm_apply(G_MIXPRE)
        make_tables(cc)
        Wk, Wk_tr = kv_proj(cc)
        wi_proj(c, Wk, Wk_tr)
        for grp in range(4):
            Wt, Wt_tr = proj_group(2048 + grp * 256, 256)
            for j in range(2):
                ps, ps_tr = proj_ps(Wt, Wt_tr, j)
                rope(ps, ps_tr, 0, qT[2 * grp + j], [qT_tr[2 * grp + j]])
        for grp in range(4):
            Wt, Wt_tr = proj_group(3584 + grp * 256, 256)
            for j in range(2):
                ps, ps_tr = proj_ps(Wt, Wt_tr, j)
                rope(ps, ps_tr, 1, qiT[2 * grp + j], [qiT_tr[2 * grp + j]])
        conv_ab(C, 0, 32)
        conv_branch()
        attn_chunk(c)
        merge_and_out()
        ffn(W1b, W2b, G_F2PRE, 16)
        store_chunk(c)

    sem_names = list(Prog.ENG) + sorted(P.dtot.keys())
    SEM = {n: es.enter_context(nc.semaphore("s_" + n)) for n in sem_names}
    block = es.enter_context(nc.Block())

    def replay(E, name):
        for waits, fn, sem, inc in P.ops[name]:
            for s, v in waits:
                E.wait_ge(SEM[s], v)
            r = fn(E)
            if isinstance(r, (list, tuple)):
                for ins in r:
                    ins.then_inc(SEM[sem], inc)
            else:
                r.then_inc(SEM[sem], inc)

    @block.tensor
    def _(E):
        replay(E, "pe")

    @block.scalar
    def _(E):
        replay(E, "act")

    @block.vector
    def _(E):
        replay(E, "dve")

    @block.gpsimd
    def _(E):
        replay(E, "pool")

    @block.sync
    def _(E):
        replay(E, "sp")
        for n in ("o0", "o1"):
            E.wait_ge(SEM[n], P.dtot[n])

    es.close()
    return nc


_NC_CACHE = {}


def _consts_for_core(half, inputs):
    cst = np.zeros((128, NCST), np.float32)

    def fm(v, ncol):
        return np.ascontiguousarray(np.asarray(v, np.float32).reshape(ncol, 128).T)
    cst[:, G_F1PRE:G_F1PRE + 16] = fm(inputs["ffn1_norm_pre"][0], 16)
    cst[:, G_F1POST:G_F1POST + 16] = fm(inputs["ffn1_norm_post"][0], 16)
    cst[:, G_MIXPRE:G_MIXPRE + 16] = fm(inputs["mix_norm_pre"][0], 16)
    cst[:, G_MIXPOST:G_MIXPOST + 16] = fm(inputs["mix_norm_post"][0], 16)
    cst[:, G_F2PRE:G_F2PRE + 16] = fm(inputs["ffn2_norm_pre"][0], 16)
    cst[:, G_F2POST:G_F2POST + 16] = fm(inputs["ffn2_norm_post"][0], 16)
    dw = np.asarray(inputs["conv_dw"][0], np.float32)
    cst[:, C_DW:C_DW + 248] = dw.reshape(31, 8, 128).transpose(2, 1, 0).reshape(128, 248)
    cst[:, C_DWB:C_DWB + 8] = fm(inputs["conv_dw_b"][0], 8)
    cst[:, C_LNG:C_LNG + 8] = fm(inputs["conv_ln_g"][0], 8)
    cst[:, C_LNB:C_LNB + 8] = fm(inputs["conv_ln_b"][0], 8)
    p = np.arange(128)
    invq = np.where(p < 32, np.power(np.float32(500000.0), -(p % 16).astype(np.float32) * np.float32(2.0 / 32)), 0.0)
    pi_ = p % 64
    invi = np.where(pi_ < 16, np.power(np.float32(500000.0), -(pi_ % 8).astype(np.float32) * np.float32(2.0 / 16)), 0.0)
    cst[:, C_INVF] = (invq / TWO_PI).astype(np.float32)
    cst[:, C_INVF + 1] = (invi / TWO_PI).astype(np.float32)
    cst[:, C_VFLAG] = float(half)
    cst[:, C_NEGBIG] = 0.0 if half else -1e30
    cst[:, C_BIS:C_BIS + NIT + 1] = -(0.5 ** np.arange(NIT + 1, dtype=np.float64)).astype(np.float32)[None, :]
    return cst


def _rmat():
    R = np.zeros((128, 256), np.float32)
    for m in range(16):
        R[m + 16, m] = -1.0
        R[m, m + 16] = 1.0
    for base in (0, 64):
        for m in range(8):
            R[base + m + 8, 128 + base + m] = -1.0
            R[base + m, 128 + base + m + 8] = 1.0
    return R


def kernel(**inputs):
    x = np.asarray(inputs["x"], np.float32)
    pos = np.asarray(inputs["positions"], np.int32)
    if "nc" not in _NC_CACHE:
        _NC_CACHE["nc"] = build_nc()
    nc = _NC_CACHE["nc"]
    wnames = ["ffn1_w1", "ffn1_w2", "w_in", "conv_w_pw", "attn_w_o", "w_out", "ffn2_w1", "ffn2_w2"]
    wts = {n: np.ascontiguousarray(np.asarray(inputs[n], np.float32)[0]) for n in wnames}
    rm = _rmat()
    in_maps = []
    for core in range(8):
        b, half = core // 2, core % 2
        own = slice(half * 2048, (half + 1) * 2048)
        xin = np.ascontiguousarray(np.concatenate([x[b, 0:2048], x[b, own]], axis=0))
        pp = np.concatenate([pos[b, 0:2048], pos[b, own]], axis=0)
        posb = np.ascontiguousarray(np.broadcast_to(pp[None, :], (128, 4096))).astype(np.int32)
        m = {"xin": xin, "posb": posb, "cst": _consts_for_core(half, inputs), "rmat": rm}
        m.update(wts)
        in_maps.append(m)
    res = run_bass_kernel_spmd(nc, in_maps, core_ids=list(range(8)))
    out = np.zeros((4, 4096, 2048), np.float32)
    for core in range(8):
        b, half = core // 2, core % 2
        out[b, half * 2048:(half + 1) * 2048] = np.asarray(res.results[core]["out"], np.float32)
    return out
```
